# Optimizing a Trainium2 kernel written in Bass

```python
import jax, jax.numpy as jnp
from jax import lax
import numpy as np

D_MODEL = 1024
BATCH = 8
SEQ = 4096
DEPTH = 4

FOX_HEADS = 16
FOX_HEAD_DIM = D_MODEL // FOX_HEADS
FOX_WIDTH = FOX_HEADS * FOX_HEAD_DIM
FOX_FORGET_BIAS = 3.0
Q_BLOCK = 128
HG_HEADS = 8
HG_KEY_DIM = 128
HG_VAL_DIM = D_MODEL // HG_HEADS
HG_KEY_WIDTH = HG_HEADS * HG_KEY_DIM
HG_VAL_WIDTH = HG_HEADS * HG_VAL_DIM
HG_CHUNK = 64
COL_SIZES = (FOX_WIDTH, FOX_WIDTH, FOX_WIDTH, FOX_HEADS, HG_KEY_WIDTH, HG_KEY_WIDTH, HG_VAL_WIDTH, HG_VAL_WIDTH, D_MODEL, D_MODEL)
D_IN = 3 * FOX_WIDTH + FOX_HEADS + 2 * HG_KEY_WIDTH + 2 * HG_VAL_WIDTH + 2 * D_MODEL
D_FF_DENSE = 2816
N_EXPERTS = 8
TOP_K = 2
D_FF_EXPERT = 3584
N_DENSE = (DEPTH + 1) // 2
N_MOE = DEPTH // 2
N_ADA = 6
EPS = 1e-6
F_MIN = 1e-20
MASK_NEG = -1e30
F32 = jnp.float32

kernel_name = "fox_hgrn2_gated_hybrid_moe_adaln"


def rms_norm(x, gain):
    xf = x.astype(F32)
    y = xf * lax.rsqrt(jnp.mean(xf * xf, axis=-1, keepdims=True) + EPS)
    return (y * gain.astype(F32)).astype(x.dtype)


def swiglu(t, w_gate, w_up, w_down):
    return (jax.nn.silu(t @ w_gate) * (t @ w_up)) @ w_down


def forgetting_attention(q, k, v, log_f):
    B, S, H, dh = q.shape
    nb = S // Q_BLOCK
    F = jnp.cumsum(log_f, axis=1)
    F_k = F.transpose(0, 2, 1)[:, :, None, :]
    qb = q.reshape(B, nb, Q_BLOCK, H, dh).transpose(1, 0, 2, 3, 4)
    Fb = F.reshape(B, nb, Q_BLOCK, H).transpose(1, 0, 3, 2)
    k_pos = jnp.arange(S)
    scale = dh ** -0.5

    def block(args):
        i, q_i, F_i = args
        s = jnp.einsum('bqhd,bkhd->bhqk', q_i, k, preferred_element_type=F32) * scale
        s = s + F_i[..., None] - F_k
        q_pos = i * Q_BLOCK + jnp.arange(Q_BLOCK)
        mask = k_pos[None, :] <= q_pos[:, None]
        p = jax.nn.softmax(jnp.where(mask, s, MASK_NEG), axis=-1)
        return jnp.einsum('bhqk,bkhd->bqhd', p.astype(v.dtype), v)

    o = lax.map(block, (jnp.arange(nb), qb, Fb))
    return o.transpose(1, 0, 2, 3, 4).reshape(B, S, H * dh)


def hgrn2_recurrence(q, k, v, log_f):
    B, S, H, dk = q.shape
    dv = v.shape[-1]
    n = S // HG_CHUNK

    def to_chunks(t):
        return t.astype(F32).reshape(B, n, HG_CHUNK, H, t.shape[-1]).transpose(1, 0, 3, 2, 4)

    qc, kc, vc, gc = to_chunks(q), to_chunks(k), to_chunks(v), to_chunks(log_f)
    causal = jnp.tril(jnp.ones((HG_CHUNK, HG_CHUNK), bool))[..., None]

    def step(state, inp):
        q_c, k_c, v_c, g_c = inp
        G = jnp.cumsum(g_c, axis=2)
        diff = G[:, :, :, None, :] - G[:, :, None, :, :]
        decay = jnp.where(causal, jnp.exp(jnp.where(causal, diff, 0.0)), 0.0)
        A = jnp.einsum('bhtk,bhsk,bhtsk->bhts', q_c, k_c, decay)
        o = jnp.einsum('bhts,bhsv->bhtv', A, v_c) + jnp.einsum('bhtk,bhkv->bhtv', q_c * jnp.exp(G), state)
        G_last = G[:, :, -1:, :]
        state = state * jnp.exp(G_last[:, :, 0, :])[..., None] + jnp.einsum('bhsk,bhsv->bhkv', k_c * jnp.exp(G_last - G), v_c)
        return state, o

    s0 = jnp.zeros((B, H, dk, dv), F32)
    _, o = lax.scan(step, s0, (qc, kc, vc, gc))
    return o.transpose(1, 0, 3, 2, 4).reshape(B, S, H, dv).astype(v.dtype)


def hybrid_mixer(h, w_in, q_gain, k_gain, f_bias, lb, o_gain, w_out):
    B, S, _ = h.shape
    proj = h @ w_in
    offs = np.cumsum(COL_SIZES)[:-1].tolist()
    fq, fk, fv, ff, hq, hf, hi, hg, ga, gb = jnp.split(proj, offs, axis=-1)
    fq = rms_norm(fq.reshape(B, S, FOX_HEADS, FOX_HEAD_DIM), q_gain)
    fk = rms_norm(fk.reshape(B, S, FOX_HEADS, FOX_HEAD_DIM), k_gain)
    fv = fv.reshape(B, S, FOX_HEADS, FOX_HEAD_DIM)
    fox_log_f = jax.nn.log_sigmoid((ff + f_bias).astype(F32))
    o_a = forgetting_attention(fq, fk, fv, fox_log_f)
    z = hf.reshape(B, S, HG_HEADS, HG_KEY_DIM).astype(F32)
    lb_h = lb.reshape(HG_HEADS, HG_KEY_DIM).astype(F32)
    hg_f = lb_h + (1.0 - lb_h) * jax.nn.sigmoid(z)
    hg_log_f = jnp.log(jnp.maximum(hg_f, F_MIN))
    hg_k = (1.0 - lb_h) * jax.nn.sigmoid(-z)
    hg_q = jax.nn.silu(hq).reshape(B, S, HG_HEADS, HG_KEY_DIM)
    hg_v = hi.reshape(B, S, HG_HEADS, HG_VAL_DIM)
    o_h = hgrn2_recurrence(hg_q, hg_k, hg_v, hg_log_f)
    o_b = (rms_norm(o_h, o_gain) * jax.nn.silu(hg.reshape(B, S, HG_HEADS, HG_VAL_DIM))).reshape(B, S, D_MODEL)
    y = jax.nn.sigmoid(ga) * o_a.astype(h.dtype) + jax.nn.sigmoid(gb) * o_b.astype(h.dtype)
    return y @ w_out


def moe_swiglu(h, w_router, b_router, w_gate, w_up, w_down):
    B, S, D = h.shape
    t = h.reshape(B * S, D)
    logits = (t @ w_router).astype(F32) + b_router.astype(F32)
    top_val, top_idx = lax.top_k(logits, TOP_K)
    top_w = jax.nn.softmax(top_val, axis=-1)
    combine = jnp.sum(jax.nn.one_hot(top_idx, N_EXPERTS, dtype=F32) * top_w[..., None], axis=1)
    out = jnp.zeros((B * S, D), F32)
    for e in range(N_EXPERTS):
        out = out + combine[:, e:e + 1] * swiglu(t, w_gate[e], w_up[e], w_down[e]).astype(F32)
    return out.astype(h.dtype).reshape(B, S, D)


def setup_inputs(seed: int = 0) -> dict:
    key = jax.random.key(seed)
    ks = jax.random.split(key, 24)

    def nrm(k, shape, s):
        return jax.random.normal(k, shape, F32) * s

    return {
        "x": nrm(ks[0], (BATCH, SEQ, D_MODEL), 1.0),
        "c": nrm(ks[1], (BATCH, D_MODEL), 1.0),
        "w_ada": nrm(ks[2], (DEPTH, D_MODEL, N_ADA * D_MODEL), 0.3 * D_MODEL ** -0.5),
        "b_ada": nrm(ks[3], (DEPTH, N_ADA * D_MODEL), 0.02),
        "norm_mix": 1.0 + nrm(ks[4], (DEPTH, D_MODEL), 0.02),
        "norm_ffn": 1.0 + nrm(ks[5], (DEPTH, D_MODEL), 0.02),
        "w_in": nrm(ks[6], (DEPTH, D_MODEL, D_IN), D_MODEL ** -0.5),
        "fox_q_gain": 1.0 + nrm(ks[7], (DEPTH, FOX_HEAD_DIM), 0.02),
        "fox_k_gain": 1.0 + nrm(ks[8], (DEPTH, FOX_HEAD_DIM), 0.02),
        "fox_f_bias": FOX_FORGET_BIAS + nrm(ks[9], (DEPTH, FOX_HEADS), 0.5),
        "hg_lb": nrm(ks[10], (DEPTH, HG_KEY_WIDTH), 0.1),
        "hg_o_gain": 1.0 + nrm(ks[11], (DEPTH, HG_VAL_DIM), 0.02),
        "w_out": nrm(ks[12], (DEPTH, D_MODEL, D_MODEL), D_MODEL ** -0.5),
        "w_router": nrm(ks[13], (N_MOE, D_MODEL, N_EXPERTS), D_MODEL ** -0.5),
        "b_router": nrm(ks[14], (N_MOE, N_EXPERTS), 0.01),
        "dense_w_gate": nrm(ks[15], (N_DENSE, D_MODEL, D_FF_DENSE), D_MODEL ** -0.5),
        "dense_w_up": nrm(ks[16], (N_DENSE, D_MODEL, D_FF_DENSE), D_MODEL ** -0.5),
        "dense_w_down": nrm(ks[17], (N_DENSE, D_FF_DENSE, D_MODEL), D_FF_DENSE ** -0.5),
        "moe_w_gate": nrm(ks[18], (N_MOE, N_EXPERTS, D_MODEL, D_FF_EXPERT), D_MODEL ** -0.5),
        "moe_w_up": nrm(ks[19], (N_MOE, N_EXPERTS, D_MODEL, D_FF_EXPERT), D_MODEL ** -0.5),
        "moe_w_down": nrm(ks[20], (N_MOE, N_EXPERTS, D_FF_EXPERT, D_MODEL), D_FF_EXPERT ** -0.5),
    }


def reference(x, c, w_ada, b_ada, norm_mix, norm_ffn, w_in, fox_q_gain, fox_k_gain, fox_f_bias, hg_lb, hg_o_gain,
              w_out, w_router, b_router, dense_w_gate, dense_w_up, dense_w_down, moe_w_gate, moe_w_up, moe_w_down):
    c_act = jax.nn.silu(c)
    lb_sm = jax.nn.softmax(hg_lb.astype(F32), axis=0)
    lb_all = jnp.cumsum(lb_sm, axis=0) - lb_sm[0:1]
    for layer in range(DEPTH):
        ada = c_act @ w_ada[layer] + b_ada[layer]
        sh1, sc1, g1, sh2, sc2, g2 = jnp.split(ada[:, None, :], N_ADA, axis=-1)
        h = rms_norm(x, norm_mix[layer]) * (1.0 + sc1) + sh1
        x = x + g1 * hybrid_mixer(h, w_in[layer], fox_q_gain[layer], fox_k_gain[layer], fox_f_bias[layer],
                                  lb_all[layer], hg_o_gain[layer], w_out[layer])
        h = rms_norm(x, norm_ffn[layer]) * (1.0 + sc2) + sh2
        if layer % 2 == 0:
            i = layer // 2
            f = swiglu(h, dense_w_gate[i], dense_w_up[i], dense_w_down[i])
        else:
            i = layer // 2
            f = moe_swiglu(h, w_router[i], b_router[i], moe_w_gate[i], moe_w_up[i], moe_w_down[i])
        x = x + g2 * f
    return x
```

```python
import numpy as np
from contextlib import ExitStack
import concourse.bass as bass
import concourse.mybir as mybir
from concourse.bass_utils import run_bass_kernel_spmd

F32 = mybir.dt.float32
BF16 = mybir.dt.bfloat16
AF = mybir.ActivationFunctionType
ALU = mybir.AluOpType
AX = mybir.AxisListType

S = 4096
D = 1024
KC = 8
NG = 8
NB = 32
DEPTH = 4
EPS = 1e-6
DFF = 2816
DFE = 3584
NE = 8
CH = 32
NCS = 512 // CH


class _Op:
    __slots__ = ("eng", "fn", "sem", "inc", "deps", "needed", "sigval", "idx")


class Sched:
    EPOCH = 12000

    def __init__(self, nc):
        self.nc = nc
        self.ops = []
        self.last_w = {}
        self.readers = {}
        self.neng = {}

    def add(self, eng, fn, r=(), w=(), sem=None):
        op = _Op()
        op.eng = eng
        op.fn = fn
        if sem is None:
            n = self.neng.get(eng, 0)
            self.neng[eng] = n + 1
            op.sem = "%s#%d" % (eng, n // self.EPOCH)
            op.inc = 1
            op.needed = False
        else:
            op.sem = sem
            op.inc = 16
            op.needed = True
        deps = {}

        def dep(o):
            if o is None:
                return
            if o.eng == "pe" and eng == "pe":
                return
            k = o.sem
            if k not in deps or deps[k].idx < o.idx:
                deps[k] = o

        for x in r:
            dep(self.last_w.get(x))
        for x in w:
            dep(self.last_w.get(x))
            rd = self.readers.get(x)
            if rd:
                for o in rd.values():
                    dep(o)
        op.idx = len(self.ops)
        op.deps = deps
        for o in deps.values():
            o.needed = True
        for x in r:
            self.readers.setdefault(x, {})[op.sem] = op
        for x in w:
            self.last_w[x] = op
            self.readers[x] = {}
        self.ops.append(op)
        return op

    def emit(self):
        nc = self.nc
        last = {}
        for op in self.ops:
            if op.fn is not None:
                last[op.sem] = op
        for op in last.values():
            op.needed = True
        cnt = {}
        for op in self.ops:
            if op.fn is not None and op.needed:
                cnt[op.sem] = cnt.get(op.sem, 0) + op.inc
                op.sigval = cnt[op.sem]
        keys = sorted(cnt.keys())
        sems = {k: nc.alloc_semaphore(name="s_" + k.replace("#", "_") + getattr(self, "uid", "")) for k in keys}
        with nc.Block() as block:

            def run(engname):
                def f(e):
                    waited = {}
                    for op in self.ops:
                        if op.eng != engname:
                            continue
                        for k, d in op.deps.items():
                            if waited.get(k, 0) >= d.sigval:
                                continue
                            e.wait_ge(sems[k], d.sigval)
                            waited[k] = d.sigval
                        if op.fn is not None:
                            ins = op.fn(e)
                            if op.needed:
                                ins.then_inc(sems[op.sem], op.inc)
                    for k in keys:
                        if waited.get(k, 0) < cnt[k]:
                            e.wait_ge(sems[k], cnt[k])
                return f

            block.tensor(run("pe"))
            block.scalar(run("act"))
            block.vector(run("dve"))
            block.gpsimd(run("pool"))
            block.sync(run("sp"))
        nc.all_engine_barrier()
        nc.clear_and_free_semaphores(list(sems.values()))
        nc.all_engine_barrier()


class MK:
    def __init__(self, n_layers=DEPTH, dbg=(), stop_after=None):
        self.n_layers = n_layers
        self.dbg = set(dbg)
        self.stop_after = stop_after
        self.nc = bass.Bass("TRN2", target_bir_lowering=False)
        self.es = ExitStack()
        self.uid = 0

    def din(self, name, shape, dt=F32):
        return self.nc.dram_tensor(name, list(shape), dt, kind="ExternalInput").ap()

    def dscr(self, name, shape, dt):
        kind = "ExternalOutput" if name in self.dbg else "Internal"
        return self.nc.dram_tensor(name, list(shape), dt, kind=kind).ap()

    def persist(self, name, shape, dt):
        return self.es.enter_context(self.nc.sbuf_tensor(name, list(shape), dt))

    CB = [float(D * EPS), float(64 * EPS), float(128 * EPS)]

    def cbias(self, v):
        i = self.CB.index(v)
        return self.T["cb"][:, i:i + 1]

    def phase(self, body):
        nc = self.nc
        with ExitStack() as es:
            s = Sched(nc)
            self.uid += 1
            u = "_u%d" % self.uid
            s.uid = u

            def sb(name, shape, dt=F32):
                return es.enter_context(nc.sbuf_tensor(name + u, list(shape), dt))

            def ps(name, shape, dt=F32):
                return es.enter_context(nc.psum_tensor(name + u, list(shape), dt))

            body(s, sb, ps)
            s.emit()

    def build(self):
        nc = self.nc
        L = self.n_layers
        shapes = {
            "xT": [D, S], "c": [128, KC], "w_ada": [DEPTH, 8, 128, KC, 768], "b_ada": [128, DEPTH, 48],
            "norm_mix": [128, DEPTH, KC], "norm_ffn": [128, DEPTH, KC], "w_in": [DEPTH, 18, 128, KC, 512],
            "w_ff": [DEPTH, 128, KC, 16], "qgain": [128, DEPTH], "kgain": [128, DEPTH], "fbias": [128, DEPTH, 16],
            "hg_lb": [128, KC, DEPTH], "ogain": [128, DEPTH], "w_out": [DEPTH, 128, KC, D],
            "w_router": [2, 128, KC, NE], "b_router": [128, 2, NE],
            "dwg": [2, 22, 128, KC, 128], "dwu": [2, 22, 128, KC, 128], "dwd": [2, 128, 22, D],
            "mwg": [2, NE, 28, 128, KC, 128], "mwu": [2, NE, 28, 128, KC, 128], "mwd": [2, NE, 128, 28, D],
            "ident": [128, 128], "bd64": [128, 128], "tri64": [64, 64], "maskneg": [128, 512],
            "resetm": [128, 512], "sel8": [8, NE, 128],
        }
        mk = self

        class _Lazy(dict):
            def __missing__(self, k):
                v = mk.din(k, shapes[k])
                self[k] = v
                return v

        I = _Lazy()
        self.I = I
        self.out = self.nc.dram_tensor("yT", [D, S], F32, kind="ExternalOutput").ap()

        Sx = {}
        Sx["XT"] = self.dscr("XT", [D, S], F32)
        Sx["QT"] = self.dscr("QT", [16, 65, S], BF16)
        Sx["KT"] = self.dscr("KT", [16, 65, S], BF16)
        Sx["V"] = self.dscr("V", [S, 16 * 65], BF16)
        Sx["QM"] = self.dscr("QM", [D, S], BF16)
        Sx["KM"] = self.dscr("KM", [D, S], BF16)
        Sx["QI"] = self.dscr("QI", [D, S], BF16)
        Sx["KL"] = self.dscr("KL", [D, S], BF16)
        Sx["VH"] = self.dscr("VH", [S, D], BF16)
        Sx["GG"] = self.dscr("GG", [D, S], BF16)
        Sx["SGA"] = self.dscr("SGA", [D, S], BF16)
        Sx["OBG"] = self.dscr("OBG", [D, S], BF16)
        self.Sx = Sx

        T = {}
        T["ident_f"] = self.persist("ident_f", [128, 128], F32)
        T["ident_b"] = self.persist("ident_b", [128, 128], BF16)
        T["ones_f"] = self.persist("ones_f", [128, 512], F32)
        T["bd64"] = self.persist("bd64_b", [128, 128], BF16)
        T["tri64"] = self.persist("tri64_f", [64, 64], F32)
        T["maskneg"] = self.persist("maskneg_b", [128, 512], BF16)
        T["resetm"] = self.persist("resetm_f", [128, 512], F32)
        T["sel8"] = self.persist("sel8_f", [8, NE, 128], F32)
        T["ADA"] = self.persist("ADA", [128, DEPTH, 48], F32)
        T["A1"] = self.persist("A1", [128, DEPTH, KC], F32)
        T["A2"] = self.persist("A2", [128, DEPTH, KC], F32)
        T["LB"] = self.persist("LB", [128, KC, DEPTH], F32)
        T["OML"] = self.persist("OML", [128, KC, DEPTH], F32)
        T["NOML"] = self.persist("NOML", [128, KC, DEPTH], F32)
        T["qgain"] = self.persist("qgain_s", [128, DEPTH], F32)
        T["kgain"] = self.persist("kgain_s", [128, DEPTH], F32)
        T["ogain"] = self.persist("ogain_s", [128, DEPTH], F32)
        T["kgain8"] = self.persist("kgain8", [128, DEPTH], F32)
        T["ogainS"] = self.persist("ogainS", [128, DEPTH], F32)
        T["fbias"] = self.persist("fbias_s", [128, DEPTH, 16], F32)
        T["brout"] = self.persist("brout_s", [128, 2, NE], F32)
        T["EGL"] = self.persist("EGL", [128, KC, S // CH], F32)
        T["cb"] = self.persist("cbias", [128, 8], F32)
        T["LF"] = self.persist("LF", [128, NB, 16], F32)
        T["NF"] = self.persist("NF", [128, NB, 16], F32)
        self.T = T

        self.phase(self.prologue)
        if self.stop_after == "prologue":
            return self.finish()
        if isinstance(self.stop_after, tuple) and self.stop_after[0] == "onlyM":
            self.l = self.stop_after[1]
            self.n_layers = self.l + 1
            self.phase(self.phase_M)
            return self.finish()
        for l in range(L):
            self.l = l
            self.phase(self.phase_A)
            if self.stop_after in (("A", l), ("A1", l)):
                return self.finish()
            self.phase(self.phase_H)
            if self.stop_after == ("H", l):
                return self.finish()
            self.phase(self.phase_T)
            if self.stop_after == ("T", l):
                return self.finish()
            self.phase(self.phase_M)
            if self.stop_after == ("M", l):
                return self.finish()
        return self.finish()

    def finish(self):
        self.es.close()
        return self.nc

    def prologue(self, s, sb, ps):
        I, T, Sx = self.I, self.T, self.Sx
        stage = sb("pstage", [128, 512], F32)
        stage2 = sb("pstage2", [128, 128], F32)
        s.add("sp", lambda e: e.dma_start(out=T["ident_f"][:], in_=I["ident"]), w=["ident_f"], sem="d0_1")
        s.add("sp", lambda e: e.dma_start(out=stage2[:], in_=I["bd64"]), w=["stage2"], sem="d0_2")
        s.add("sp", lambda e: e.dma_start(out=T["tri64"][:], in_=I["tri64"]), w=["tri64"], sem="d0_3")
        s.add("sp", lambda e: e.dma_start(out=stage[:], in_=I["maskneg"]), w=["stage"], sem="d0_4")
        s.add("sp", lambda e: e.dma_start(out=T["resetm"][:], in_=I["resetm"]), w=["resetm"], sem="d0_5")
        s.add("sp", lambda e: e.dma_start(out=T["sel8"][:], in_=I["sel8"]), w=["sel8"], sem="d0_6")
        s.add("dve", lambda e: e.tensor_copy(T["ident_b"][:], T["ident_f"][:]), r=["ident_f"], w=["ident_b"])
        s.add("dve", lambda e: e.tensor_copy(T["bd64"][:], stage2[:]), r=["stage2"], w=["bd64"])
        s.add("dve", lambda e: e.tensor_copy(T["maskneg"][:], stage[:]), r=["stage"], w=["maskneg"])
        s.add("pool", lambda e: e.memset(T["ones_f"][:], 1.0), w=["ones_f"])
        for i, v in enumerate(self.CB):
            s.add("pool", (lambda i, v: lambda e: e.memset(T["cb"][:, i:i + 1], v))(i, v), w=["cb"])
        for nm in ["qgain", "kgain", "ogain", "fbias"]:
            s.add("sp", (lambda nm: lambda e: e.dma_start(out=T[nm][:], in_=I[nm]))(nm), w=[nm], sem="d0_" + nm)
        s.add("sp", lambda e: e.dma_start(out=T["brout"][:], in_=I["b_router"]), w=["brout"], sem="d0_8")
        s.add("dve", lambda e: e.tensor_scalar(out=T["kgain8"][:], in0=T["kgain"][:], scalar1=8.0, scalar2=None,
                                               op0=ALU.mult), r=["kgain"], w=["kgain8"])
        s.add("dve", lambda e: e.tensor_scalar(out=T["ogainS"][:], in0=T["ogain"][:], scalar1=float(128 ** 0.5),
                                               scalar2=None, op0=ALU.mult), r=["ogain"], w=["ogainS"])
        nmix = sb("nmix", [128, DEPTH, KC], F32)
        nffn = sb("nffn", [128, DEPTH, KC], F32)
        bada = sb("bada", [128, DEPTH, 48], F32)
        cin = sb("cin", [128, KC], F32)
        cact = sb("cact", [128, KC], F32)
        lbin = sb("lbin", [128, KC, DEPTH], F32)
        s.add("sp", lambda e: e.dma_start(out=nmix[:], in_=I["norm_mix"]), w=["nmix"], sem="d0_9")
        s.add("sp", lambda e: e.dma_start(out=nffn[:], in_=I["norm_ffn"]), w=["nffn"], sem="d0_10")
        s.add("sp", lambda e: e.dma_start(out=bada[:], in_=I["b_ada"]), w=["bada"], sem="d0_11")
        s.add("sp", lambda e: e.dma_start(out=cin[:], in_=I["c"]), w=["cin"], sem="d0_12")
        s.add("sp", lambda e: e.dma_start(out=lbin[:], in_=I["hg_lb"]), w=["lbin"], sem="d0_13")
        s.add("sp", lambda e: e.dma_start(out=Sx["XT"], in_=I["xT"]), w=["XT"], sem="d1")
        onesb = sb("onesb", [16, S], BF16)
        s.add("pool", lambda e: e.memset(onesb[:], 1.0), w=["onesb"])
        s.add("sp", lambda e: e.dma_start(out=Sx["KT"][:, 64, :], in_=onesb[:]), r=["onesb"], w=["KT64"], sem="d1b")
        s.add("act", lambda e: e.activation(out=cact[:], in_=cin[:], func=AF.Silu), r=["cin"], w=["cact"])
        wa = [sb("wa%d" % i, [128, KC, 768], F32) for i in range(2)]
        pada = ps("pada", [128, 512], F32)
        n = 0
        for l in range(DEPTH):
            for blk in range(8):
                b = n % 2
                n += 1
                s.add("sp", (lambda b, l, blk: lambda e: e.dma_start(out=wa[b][:], in_=I["w_ada"][l, blk]))(b, l, blk),
                      w=["wa%d" % b], sem="dwa%d" % b)
                for m in range(6):
                    col = l * 48 + blk * 6 + m
                    for kc in range(KC):
                        s.add("pe", (lambda b, m, kc, col: lambda e: e.matmul(
                            pada[:, col:col + 1], wa[b][:, kc, m * 128:(m + 1) * 128], cact[:, kc:kc + 1],
                            start=(kc == 0), stop=(kc == KC - 1)))(b, m, kc, col),
                            r=["wa%d" % b, "cact"], w=["pada"])
        s.add("dve", lambda e: e.tensor_tensor(
            out=T["ADA"][:].rearrange("p l m -> p (l m)"), in0=pada[:, 0:DEPTH * 48],
            in1=bada[:].rearrange("p l m -> p (l m)"), op=ALU.add), r=["pada", "bada"], w=["ADA"])
        tmpA = sb("tmpA", [128, DEPTH, KC], F32)
        s.add("dve", lambda e: e.tensor_scalar(out=tmpA[:], in0=T["ADA"][:, :, 8:16], scalar1=1.0, scalar2=32.0,
                                               op0=ALU.add, op1=ALU.mult), r=["ADA"], w=["tmpA"])
        s.add("dve", lambda e: e.tensor_tensor(out=T["A1"][:], in0=tmpA[:], in1=nmix[:], op=ALU.mult),
              r=["tmpA", "nmix"], w=["A1"])
        tmpB = sb("tmpB", [128, DEPTH, KC], F32)
        s.add("dve", lambda e: e.tensor_scalar(out=tmpB[:], in0=T["ADA"][:, :, 32:40], scalar1=1.0, scalar2=32.0,
                                               op0=ALU.add, op1=ALU.mult), r=["ADA"], w=["tmpB"])
        s.add("dve", lambda e: e.tensor_tensor(out=T["A2"][:], in0=tmpB[:], in1=nffn[:], op=ALU.mult),
              r=["tmpB", "nffn"], w=["A2"])
        lbe = sb("lbe", [128, KC, DEPTH], F32)
        lbs = sb("lbs", [128, KC], F32)
        lbr = sb("lbr", [128, KC], F32)
        lbsm = sb("lbsm", [128, KC, DEPTH], F32)
        s.add("act", lambda e: e.activation(out=lbe[:], in_=lbin[:], func=AF.Exp), r=["lbin"], w=["lbe"])
        s.add("dve", lambda e: e.tensor_reduce(out=lbs[:], in_=lbe[:], axis=AX.X, op=ALU.add), r=["lbe"], w=["lbs"])
        s.add("dve", lambda e: e.reciprocal(lbr[:], lbs[:]), r=["lbs"], w=["lbr"])
        s.add("dve", lambda e: e.tensor_tensor(out=lbsm[:], in0=lbe[:],
                                               in1=lbr[:].unsqueeze(2).broadcast_to([128, KC, DEPTH]), op=ALU.mult),
              r=["lbe", "lbr"], w=["lbsm"])
        s.add("pool", lambda e: e.memset(T["LB"][:, :, 0:1], 0.0), w=["LB"])
        for l in range(1, DEPTH):
            s.add("dve", (lambda l: lambda e: e.tensor_tensor(
                out=T["LB"][:, :, l:l + 1], in0=T["LB"][:, :, l - 1:l], in1=lbsm[:, :, l:l + 1], op=ALU.add))(l),
                r=["LB", "lbsm"], w=["LB"])
        s.add("dve", lambda e: e.tensor_scalar(out=T["OML"][:], in0=T["LB"][:], scalar1=-1.0, scalar2=1.0,
                                               op0=ALU.mult, op1=ALU.add), r=["LB"], w=["OML"])
        s.add("dve", lambda e: e.tensor_scalar(out=T["NOML"][:], in0=T["LB"][:], scalar1=1.0, scalar2=-1.0,
                                               op0=ALU.mult, op1=ALU.add), r=["LB"], w=["NOML"])

    def norm_mod(self, s, sb, ps, hT, Atab, Aname, Bcol0, l, h32=None):
        T, Sx = self.T, self.Sx
        xg = [sb("xg%d" % i, [128, KC, 512], F32) for i in range(2)]
        sq = [sb("sq%d" % i, [128, 512], F32) for i in range(2)]
        tmp = [sb("nt%d" % i, [128, 512], F32) for i in range(2)]
        rstd = [sb("rstd%d" % i, [128, 512], F32) for i in range(2)]
        pss = [ps("pssq%d" % i, [128, 512], F32) for i in range(2)]
        xt = Sx["XT"].rearrange("(kc p) t -> p kc t", p=128)
        def do(g):
            b = g % 2
            s.add("sp", (lambda b, g: lambda e: e.dma_start(out=xg[b][:], in_=xt[:, :, g * 512:(g + 1) * 512]))(b, g),
                  r=["XT"], w=["xg%d" % b], sem="dxg%d" % b)
            for kc in range(KC):
                q = kc % 2
                s.add("pool", (lambda b, kc, q: lambda e: e.tensor_tensor(
                    out=sq[q][:], in0=xg[b][:, kc, :], in1=xg[b][:, kc, :], op=ALU.mult))(b, kc, q),
                    r=["xg%d" % b], w=["sq%d" % q])
                s.add("pe", (lambda b, kc, q: lambda e: e.matmul(
                    pss[b][:], T["ones_f"][:, 0:128], sq[q][:], start=(kc == 0), stop=(kc == KC - 1)))(b, kc, q),
                    r=["sq%d" % q, "ones_f"], w=["pss%d" % b])
            s.add("act", (lambda b: lambda e: e.activation(
                out=rstd[b][:], in_=pss[b][:], func=AF.Ln, bias=self.cbias(float(D * EPS))))(b),
                r=["pss%d" % b], w=["rstd%d" % b])
            s.add("act", (lambda b: lambda e: e.activation(
                out=rstd[b][:], in_=rstd[b][:], func=AF.Exp, scale=-0.5))(b),
                r=["rstd%d" % b], w=["rstd%d" % b])
            for kc in range(KC):
                q = kc % 2
                s.add("dve", (lambda b, kc, q: lambda e: e.tensor_tensor(
                    out=tmp[q][:], in0=xg[b][:, kc, :], in1=rstd[b][:], op=ALU.mult))(b, kc, q),
                    r=["xg%d" % b, "rstd%d" % b], w=["nt%d" % q])
                if h32 is None:
                    s.add("act", (lambda g, kc, q: lambda e: e.activation(
                        out=hT[:, kc, g * 512:(g + 1) * 512], in_=tmp[q][:], func=AF.Identity,
                        bias=T["ADA"][:, l, Bcol0 + kc:Bcol0 + kc + 1], scale=Atab[:, l, kc:kc + 1]))(g, kc, q),
                        r=["nt%d" % q, "ADA", Aname], w=["hT%d" % g])
        return xg, do

    def phase_A(self, s, sb, ps):
        I, T, Sx = self.I, self.T, self.Sx
        l = self.l
        hT = sb("hT", [128, KC, S], BF16)
        xg, norm_group = self.norm_mod(s, sb, ps, hT, T["A1"], "A1", 0, l)
        norm_group(0)
        if self.stop_after == ("A1", l):
            for g in range(1, NG):
                norm_group(g)
            dbg = self.dscr("HT", [D, S], BF16)
            s.add("sp", lambda e: e.dma_start(out=dbg.rearrange("(kc p) t -> p kc t", p=128), in_=hT[:]),
                  r=["hT%d" % g for g in range(NG)], w=["HTd"], sem="dbg")
            return
        X = Ops(s)
        wb = Rot(sb, "wb", 2, [128, KC, 512], BF16)
        pacc = Rot(ps, "pacc", 4, [128, 512], F32)
        pn = Rot(ps, "pn", 2, [128, 512], F32)
        stg = Rot(sb, "stg", 8, [128, 512], BF16)
        f32t = Rot(sb, "ft", 12, [128, 512], F32)
        sqb = Rot(sb, "sqb", 2, [128, 512], BF16)
        vst = Rot(sb, "vst", 2, [128, 8, 65], BF16)
        for t_, n_ in vst.t:
            X.memset("pool", t_[:], 1.0, w=[n_])
        wff = sb("wff", [128, KC, 16], BF16)
        X.dma("pool", wff[:], I["w_ff"][l], r=[], w=["wff"])
        tsl = lambda g: slice(g * 512, (g + 1) * 512)

        def mm_fm(w_, wn, ci, g):
            p_, pn_ = pacc.get()
            for kc in range(KC):
                X.mm(p_[:], w_[:, kc, ci * 128:(ci + 1) * 128], hT[:, kc, tsl(g)], kc == 0, kc == KC - 1,
                     r=[wn, "hT%d" % g], w=[pn_])
            return p_, pn_

        def rsq(src, srcn, cb):
            r_, rn = f32t.get()
            X.act(r_[:], src, AF.Ln, r=[srcn, "cb"], w=[rn], bias=self.cbias(cb))
            X.act(r_[:], r_[:], AF.Exp, r=[rn], w=[rn], scale=-0.5)
            return r_, rn

        for gi in range(18):
            w_, wn = wb.get()
            X.dma("pool", w_[:], I["w_in"][l, gi], r=[], w=[wn])
            if gi < 4:
                isq = gi < 2
                dst = Sx["QT"] if isq else Sx["KT"]
                gcol = T["qgain"][:, l:l + 1] if isq else T["kgain8"][:, l:l + 1]
                for g in range(NG):
                    if gi == 0 and g + 1 < NG:
                        norm_group(g + 1)
                    for ci in range(4):
                        fc = (gi % 2) * 4 + ci
                        p_, pn_ = mm_fm(w_, wn, ci, g)
                        q_, qn_ = sqb.get()
                        X.act(q_[:], p_[:], AF.Square, r=[pn_], w=[qn_])
                        n_, nn_ = pn.get()
                        X.mm(n_[:], T["bd64"][:], q_[:], True, True, r=[qn_, "bd64"], w=[nn_])
                        r_, rn = rsq(n_[:], nn_, float(64 * EPS))
                        o_, on_ = stg.get()
                        X.stt("dve", o_[:], p_[:], gcol, r_[:], ALU.mult, ALU.mult, r=[pn_, rn, "gains"], w=[on_])
                        for i in range(2):
                            X.dma("sp", dst[2 * fc + i, 0:64, tsl(g)], o_[i * 64:(i + 1) * 64, :], r=[on_],
                                  w=["QT" if isq else "KT"], key="d_" + on_)
            elif gi < 8:
                for tb in range(NB):
                    p_, pn_ = pacc.get()
                    for kc in range(KC):
                        X.mm(p_[:], hT[:, kc, tb * 128:(tb + 1) * 128], w_[:, kc, :], kc == 0, kc == KC - 1,
                             r=[wn, "hT%d" % (tb // 4)], w=[pn_])
                    if gi < 6:
                        v_, vn_ = vst.get()
                        X.act(v_[:, :, 0:64], p_[:].rearrange("p (h d) -> p h d", d=64), AF.Copy, r=[pn_], w=[vn_])
                        h0 = (gi - 4) * 8
                        X.dma("sp", Sx["V"][tb * 128:(tb + 1) * 128, h0 * 65:(h0 + 8) * 65],
                              v_[:].rearrange("p h d -> p (h d)"), r=[vn_], w=["V"], key="d_" + vn_)
                    else:
                        o_, on_ = stg.get()
                        X.copy("dve", o_[:], p_[:], r=[pn_], w=[on_])
                        c0 = (gi - 6) * 512
                        X.dma("sp", Sx["VH"][tb * 128:(tb + 1) * 128, c0:c0 + 512], o_[:], r=[on_], w=["VH"],
                              key="d_" + on_)
                if gi == 5:
                    pf_, pfn_ = pacc.get()
                    for tb in range(NB):
                        for kc in range(KC):
                            X.mm(pf_[:, tb * 16:(tb + 1) * 16], hT[:, kc, tb * 128:(tb + 1) * 128], wff[:, kc, :],
                                 kc == 0, kc == KC - 1, r=["wff", "hT%d" % (tb // 4)], w=[pfn_])
                    t_, tn_ = f32t.get()
                    X.tt("dve", t_[:].rearrange("p (b h) -> p b h", h=16), pf_[:].rearrange("p (b h) -> p b h", h=16),
                         T["fbias"][:, l:l + 1, :].broadcast_to([128, NB, 16]), ALU.add, r=[pfn_, "fbias"], w=[tn_])
                    X.act(t_[:], t_[:], AF.Sigmoid, r=[tn_], w=[tn_])
                    X.act(T["LF"][:].rearrange("p b h -> p (b h)"), t_[:], AF.Ln, r=[tn_], w=["LF"])
            elif gi < 12:
                for g in range(NG):
                    for jj in range(2):
                        j = (gi - 8) * 2 + jj
                        pz, pzn = mm_fm(w_, wn, 2 * jj, g)
                        pq, pqn = mm_fm(w_, wn, 2 * jj + 1, g)
                        sig, sign = f32t.get()
                        X.act(sig[:], pz[:], AF.Sigmoid, r=[pzn], w=[sign])
                        f_, fn_ = f32t.get()
                        X.ts("dve", f_[:], sig[:], T["OML"][:, j, l:l + 1], T["LB"][:, j, l:l + 1], ALU.mult, ALU.add,
                             r=[sign, "LBt"], w=[fn_])
                        X.s.add("pool", (lambda f_: lambda e: e.tensor_scalar_max(f_[:], f_[:], 1e-20))(f_), r=[fn_], w=[fn_])
                        X.act(f_[:], f_[:], AF.Ln, r=[fn_], w=[fn_])
                        kk, kkn = f32t.get()
                        X.ts("dve", kk[:], sig[:], T["NOML"][:, j, l:l + 1], T["OML"][:, j, l:l + 1], ALU.mult, ALU.add,
                             r=[sign, "LBt"], w=[kkn])
                        G_, Gn = f32t.get()
                        X.s.add("dve", (lambda G_, f_: lambda e: e.tensor_tensor_scan(
                            out=G_[:], data0=T["resetm"][:], data1=f_[:], initial=0.0, op0=ALU.mult, op1=ALU.add))(G_, f_),
                            r=[fn_, "resetm"], w=[Gn])
                        G3 = G_[:].rearrange("p (c i) -> p c i", i=CH)
                        d1, d1n = f32t.get()
                        X.tt("dve", d1[:].rearrange("p (c i) -> p c i", i=CH), G3, G3[:, :, CH // 2 - 1:CH // 2].broadcast_to([128, NCS, CH]),
                             ALU.subtract, r=[Gn], w=[d1n])
                        d3, d3n = f32t.get()
                        X.tt("dve", d3[:].rearrange("p (c i) -> p c i", i=CH), G3, G3[:, :, CH - 1:CH].broadcast_to([128, NCS, CH]),
                             ALU.subtract, r=[Gn], w=[d3n])
                        X.act(T["EGL"][:, j, g * NCS:(g + 1) * NCS], G3[:, :, CH - 1], AF.Exp, r=[Gn], w=["EGL"])
                        e1, e1n = f32t.get()
                        X.act(e1[:], d1[:], AF.Exp, r=[d1n], w=[e1n])
                        X.act(d1[:], d1[:], AF.Exp, r=[d1n], w=[d1n], scale=-1.0)
                        X.act(d3[:], d3[:], AF.Exp, r=[d3n], w=[d3n], scale=-1.0)
                        X.act(G_[:], G_[:], AF.Exp, r=[Gn], w=[Gn])
                        X.act(sig[:], pq[:], AF.Silu, r=[pqn, sign], w=[sign])
                        rows = slice(j * 128, (j + 1) * 128)
                        for (a_, an_, b_, bn_, dstn) in [(sig, sign, e1, e1n, "QM"), (sig, sign, G_, Gn, "QI"),
                                                         (kk, kkn, d1, d1n, "KM"), (kk, kkn, d3, d3n, "KL")]:
                            o_, on_ = stg.get()
                            X.tt("dve", o_[:], a_[:], b_[:], ALU.mult, r=[an_, bn_], w=[on_])
                            X.dma("sp", Sx[dstn][rows, tsl(g)], o_[:], r=[on_], w=[dstn], key="d_" + on_)
            elif gi < 16:
                for g in range(NG):
                    for jj in range(2):
                        j = (gi - 12) * 2 + jj
                        pg_, pgn = mm_fm(w_, wn, 2 * jj, g)
                        pb_, pbn = mm_fm(w_, wn, 2 * jj + 1, g)
                        a_, an_ = f32t.get()
                        X.act(a_[:], pg_[:], AF.Silu, r=[pgn], w=[an_])
                        b_, bn_ = f32t.get()
                        X.act(b_[:], pb_[:], AF.Sigmoid, r=[pbn], w=[bn_])
                        o_, on_ = stg.get()
                        X.tt("pool", o_[:], a_[:], b_[:], ALU.mult, r=[an_, bn_], w=[on_])
                        X.dma("sp", Sx["GG"][j * 128:(j + 1) * 128, tsl(g)], o_[:], r=[on_], w=["GG"], key="d_" + on_)
            else:
                for g in range(NG):
                    for ci in range(4):
                        fc = (gi - 16) * 4 + ci
                        p_, pn_ = mm_fm(w_, wn, ci, g)
                        o_, on_ = stg.get()
                        X.act(o_[:], p_[:], AF.Sigmoid, r=[pn_], w=[on_])
                        X.dma("sp", Sx["SGA"][fc * 128:(fc + 1) * 128, tsl(g)], o_[:], r=[on_], w=["SGA"], key="d_" + on_)

        lft = xg[0][0:16].rearrange("p k t -> p (k t)")
        ft = xg[1][0:16].rearrange("p k t -> p (k t)")
        rrow = sb("rrow", [16, S], BF16)
        for q4 in range(8):
            p_, pn_ = pacc.get()
            for i in range(4):
                tb = q4 * 4 + i
                X.s.add("pe", (lambda p_, i, tb: lambda e: e.transpose(
                    p_[0:16, i * 128:(i + 1) * 128], T["LF"][:, tb, :], T["ident_f"][:]))(p_, i, tb),
                    r=["LF", "ident_f"], w=[pn_])
            X.copy("dve", lft[:, q4 * 512:(q4 + 1) * 512], p_[0:16, :], r=[pn_], w=["xg0"])
        for q4 in range(8):
            ini = 0.0 if q4 == 0 else ft[:, q4 * 512 - 1:q4 * 512]
            X.s.add("dve", (lambda q4, ini: lambda e: e.tensor_tensor_scan(
                out=ft[:, q4 * 512:(q4 + 1) * 512], data0=T["ones_f"][0:16, :], data1=lft[:, q4 * 512:(q4 + 1) * 512],
                initial=ini, op0=ALU.mult, op1=ALU.add))(q4, ini), r=["xg0", "xg1"], w=["xg1"])
        X.memset("pool", rrow[:, 0:128], 0.0, w=["rrow"])
        X.copy("dve", rrow[:, 128:].rearrange("h (b i) -> h b i", i=128),
               ft[:, 0:31 * 128].rearrange("h (b i) -> h b i", i=128)[:, :, 127:128].broadcast_to([16, 31, 128]),
               r=["xg1", "rrow"], w=["rrow"])
        X.dma("sp", Sx["QT"][:, 64, :], rrow[:], r=["rrow"], w=["QT"], key="d_rrow")
        p_, pn_ = pacc.get()
        for tb in range(NB):
            X.s.add("pe", (lambda p_, tb: lambda e: e.transpose(
                p_[:, tb * 16:(tb + 1) * 16], ft[:, tb * 128:(tb + 1) * 128], T["ident_f"][0:16, 0:16]))(p_, tb),
                r=["xg1", "ident_f"], w=[pn_])
        X.s.add("dve", (lambda p_: lambda e: e.tensor_scalar(
            out=T["NF"][:].rearrange("p b h -> p (b h)"), in0=p_[:], scalar1=-1.0, scalar2=None, op0=ALU.mult))(p_),
            r=[pn_], w=["NF"])
        if "NFd" in self.dbg:
            nfd = self.dscr("NFd", [128, NB * 16], F32)
            X.dma("sp", nfd, T["NF"][:].rearrange("p b h -> p (b h)"), r=["NF"], w=["NFd"])
            egd = self.dscr("EGLd", [128, KC * (S // CH)], F32)
            X.dma("sp", egd, T["EGL"][:].rearrange("p j c -> p (j c)"), r=["EGL"], w=["EGLd"])


    def phase_H(self, s, sb, ps):
        I, T, Sx = self.I, self.T, self.Sx
        l = self.l
        X = Ops(s)
        ones_b = sb("ones_b", [128, 128], BF16)
        X.memset("pool", ones_b[:], 1.0, w=["ones_b"])
        segb = {nm: Rot(sb, "sg" + nm, 2, [128, KC, 512], BF16) for nm in ["QM", "KM", "QI", "KL", "GG"]}
        HV = NCS // 2
        vsg = Rot(sb, "vsg", 2, [CH, HV, D], BF16)
        St = sb("St", [128, KC, 128], F32)
        Sb = Rot(sb, "Sb", 2, [128, KC, 128], BF16)
        X.memset("pool", St[:], 0.0, w=["St%d" % j for j in range(KC)])
        sb0, sb0n = Sb.get()
        X.memset("pool", sb0[:], 0.0, w=[sb0n])
        pA = Rot(ps, "pA", 2, [CH, KC * CH], F32)
        pK = Rot(ps, "pK", 2, [CH, 1024], BF16)
        pU = Rot(ps, "pU", 2, [128, 512], F32)
        pO = Rot(ps, "pO", 1, [128, 512], F32)
        pN = Rot(ps, "pN", 1, [128, 512], F32)
        Am = Rot(sb, "Am", 2, [CH, KC, CH], BF16)
        kh = Rot(sb, "kh", 2, [CH, KC, 128], BF16)
        obuf = Rot(sb, "obuf", 2, [128, KC, 512], F32)
        osq = Rot(sb, "osq", 2, [128, 512], BF16)
        rt = Rot(sb, "hrt", 2, [128, 512], F32)
        t1 = Rot(sb, "ht1", 2, [128, 512], F32)
        ostg = Rot(sb, "ostg", 3, [128, 512], BF16)
        fmv = lambda nm: Sx[nm].rearrange("(j p) t -> p j t", p=128)
        cur = None

        def load_seg(sg):
            d = {}
            for nm in ["QM", "KM", "QI", "KL", "GG"]:
                t_, n_ = segb[nm].get()
                X.dma("sp", t_[:], fmv(nm)[:, :, sg * 512:(sg + 1) * 512], r=[nm], w=[n_])
                d[nm] = (t_, n_)
            return d

        def load_v(hv):
            v_, vn_ = vsg.get()
            X.dma("sp", v_[:], Sx["VH"][hv * 256:(hv + 1) * 256, :].rearrange("(c s) d -> s c d", s=CH), r=["VH"], w=[vn_])
            return (v_, vn_)

        def front(d, c):
            cs = slice(c * CH, (c + 1) * CH)
            pa, pan = pA.get()
            for j in range(KC):
                X.mm(pa[:, j * CH:(j + 1) * CH], d["KM"][0][:, j, cs], d["QM"][0][:, j, cs], True, True,
                     r=[d["KM"][1], d["QM"][1]], w=[pan])
            am, amn = Am.get()
            X.tt("dve", am[:], pa[:].rearrange("s (j t) -> s j t", t=CH),
                 T["tri64"][0:CH, 0:CH].unsqueeze(1).broadcast_to([CH, KC, CH]), ALU.mult, r=[pan], w=[amn])
            pk, pkn = pK.get()
            for j in range(KC):
                X.s.add("pe", (lambda pk, j, cs: lambda e: e.transpose(
                    pk[:, j * 128:(j + 1) * 128], d["KL"][0][:, j, cs], T["ident_b"][:]))(pk, j, cs),
                    r=[d["KL"][1]], w=[pkn])
            k_, kn_ = kh.get()
            X.act(k_[:].rearrange("s j k -> s (j k)"), pk[:], AF.Copy, r=[pkn], w=[kn_])
            return (am, amn, k_, kn_)

        sbc = (sb0, sb0n)
        nxt = load_seg(0)
        vnext = load_v(0)
        fr = front(nxt, 0)
        for sg in range(NG):
            d = nxt
            if sg + 1 < NG:
                nxt = load_seg(sg + 1)
            ob, obn = obuf.get()
            for c in range(NCS):
                cs = slice(c * CH, (c + 1) * CH)
                am, amn, k_, kn_ = fr
                if c % HV == 0:
                    v_, vn_ = vnext
                    hv = sg * 2 + c // HV
                    if hv + 1 < 2 * NG:
                        vnext = load_v(hv + 1)
                cv = c % HV
                pus = [pU.get(), pU.get()]
                for j in range(KC):
                    pu, pun = pus[j // 4]
                    X.mm(pu[:, (j % 4) * 128:(j % 4 + 1) * 128], k_[:, j, :], v_[:, cv, j * 128:(j + 1) * 128], True, True,
                         r=[kn_, vn_], w=[pun])
                if c + 1 < NCS:
                    fr = front(d, c + 1)
                elif sg + 1 < NG:
                    fr = front(nxt, 0)
                po, pon = pO.get()
                for j in range(KC):
                    X.mm(po[:, j * CH:(j + 1) * CH], v_[:, cv, j * 128:(j + 1) * 128], am[:, j, :], True, False,
                         r=[vn_, amn], w=[pon])
                    X.mm(po[:, j * CH:(j + 1) * CH], sbc[0][:, j, :], d["QI"][0][:, j, cs], False, True,
                         r=[sbc[1], d["QI"][1]], w=[pon])
                X.act(ob[:, :, cs], po[:, 0:KC * CH].rearrange("p (j t) -> p j t", t=CH), AF.Copy, r=[pon], w=[obn])
                cidx = sg * NCS + c
                for j in range(KC):
                    pu, pun = pus[j // 4]
                    X.stt("dve", St[:, j, :], St[:, j, :], T["EGL"][:, j, cidx:cidx + 1],
                          pu[:, (j % 4) * 128:(j % 4 + 1) * 128], ALU.mult, ALU.add, r=["St%d" % j, pun], w=["St%d" % j])
                sbc = Sb.get()
                X.copy("pool", sbc[0][:], St[:], r=["St%d" % j for j in range(KC)], w=[sbc[1]])
            for j in range(KC):
                q_, qn_ = osq.get()
                X.tt("pool", q_[:], ob[:, j, :], ob[:, j, :], ALU.mult, r=[obn], w=[qn_])
                n_, nn_ = pN.get()
                X.mm(n_[:], ones_b[:], q_[:], True, True, r=[qn_, "ones_b"], w=[nn_])
                r_, rn_ = rt.get()
                X.act(r_[:], n_[:], AF.Ln, r=[nn_], w=[rn_], bias=self.cbias(float(128 * EPS)))
                X.act(r_[:], r_[:], AF.Exp, r=[rn_], w=[rn_], scale=-0.5)
                a_, an_ = t1.get()
                X.stt("dve", a_[:], ob[:, j, :], T["ogainS"][:, l:l + 1], r_[:], ALU.mult, ALU.mult, r=[obn, rn_], w=[an_])
                o_, on_ = ostg.get()
                X.tt("dve", o_[:], a_[:], d["GG"][0][:, j, :], ALU.mult, r=[an_, d["GG"][1]], w=[on_])
                X.dma("sp", Sx["OBG"][j * 128:(j + 1) * 128, sg * 512:(sg + 1) * 512], o_[:], r=[on_], w=["OBG"],
                      key="d_" + on_)

    def phase_T(self, s, sb, ps):
        I, T, Sx = self.I, self.T, self.Sx
        l = self.l
        X = Ops(s)
        Vsb = sb("Vsb", [128, NB, 16 * 65], BF16)
        for q in range(4):
            X.dma("sp", Vsb[:, q * 8:(q + 1) * 8, :],
                  Sx["V"][q * 1024:(q + 1) * 1024, :].rearrange("(b p) c -> p b c", p=128), r=["V"], w=["Vsb%d" % q],
                  key="d_Vsb%d" % q)
        WO = sb("WO", [128, KC, D], BF16)
        X.dma("pool", WO[:], I["w_out"][l], r=[], w=["WO"])
        ktb = Rot(sb, "ktb", 2, [65, S], BF16)
        qtb = Rot(sb, "qtb", 2, [65, 512], BF16)
        ptb = Rot(sb, "ptb", 4, [128, 512], BF16)
        oab = Rot(sb, "oab", 2, [128, 4, D], BF16)
        rec = Rot(sb, "rec", 2, [128, 4], F32)
        pS = Rot(ps, "pS", 3, [128, 512], F32)
        pOo = Rot(ps, "pOo", 2, [128, 4, 65], F32)
        pTr = Rot(ps, "pTr", 1, [128, 512], BF16)
        pY = Rot(ps, "pY", 2, [128, 512], F32)
        sga = Rot(sb, "sga", 1, [128, KC, 512], BF16)
        obg = Rot(sb, "obg", 1, [128, KC, 512], BF16)
        mt = Rot(sb, "mt", 2, [128, 512], F32)
        yT = Rot(sb, "yT", 1, [128, KC, 512], BF16)
        xg = Rot(sb, "xgT", 1, [128, KC, 512], F32)
        fmv = lambda ap: ap.rearrange("(j p) t -> p j t", p=128)
        g1c = 16
        for G in range(NG):
            nk = 4 * G + 4
            oa, oan = oab.get()
            LA = 2
            tiles = [(h, j) for h in range(16) for j in range(nk)]
            hd = {}

            def load_head(h):
                kt, ktn = ktb.get()
                X.dma("sp", kt[:, 0:nk * 128], Sx["KT"][h, :, 0:nk * 128], r=["KT", "KT64"], w=[ktn])
                qt, qtn = qtb.get()
                X.dma("sp", qt[:], Sx["QT"][h, :, G * 512:(G + 1) * 512], r=["QT"], w=[qtn])
                hd[h] = [kt, ktn, qt, qtn, None]

            def qk(h, j):
                if j == 0:
                    if h + 1 < 16:
                        load_head(h + 1)
                kt, ktn, qt, qtn, _ = hd[h]
                nq0 = max(0, j - 4 * G)
                N = 512 - 128 * nq0
                p_, pn_ = pS.get()
                diag = j >= 4 * G
                if diag:
                    X.mm(p_[:, 0:N], T["ident_b"][:], T["maskneg"][:, 0:N], True, False, r=[], w=[pn_])
                X.mm(p_[:, 0:N], kt[:, j * 128:(j + 1) * 128], qt[:, nq0 * 128:512], not diag, True,
                     r=[ktn, qtn], w=[pn_])
                return (p_, pn_, nq0, N)

            load_head(0)
            pend = [qk(*tiles[i]) for i in range(min(LA, len(tiles)))]
            for i, (h, j) in enumerate(tiles):
                if i + LA < len(tiles):
                    pend.append(qk(*tiles[i + LA]))
                p_, pn_, nq0, N = pend.pop(0)
                if j == 0:
                    hd[h][4] = pOo.get()
                po, pon = hd[h][4]
                pt, ptn = ptb.get()
                X.act(pt[:, 0:N], p_[:, 0:N], AF.Exp, r=[pn_, "NF"], w=[ptn], bias=T["NF"][:, j, h:h + 1])
                for qb in range(nq0, 4):
                    X.mm(po[:, qb, :], pt[:, (qb - nq0) * 128:(qb - nq0 + 1) * 128], Vsb[:, j, h * 65:(h + 1) * 65],
                         j == 0 and qb == 0, j == 4 * G + qb, r=[ptn, "Vsb%d" % (j // 8)], w=[pon], skip=True)
                if j == nk - 1:
                    rc, rcn = rec.get()
                    X.s.add("dve", (lambda rc, po: lambda e: e.reciprocal(rc[:], po[:, :, 64]))(rc, po), r=[pon], w=[rcn])
                    X.tt("dve", oa[:, :, h * 64:(h + 1) * 64], po[:, :, 0:64], rc[:].unsqueeze(2).broadcast_to([128, 4, 64]),
                         ALU.mult, r=[pon, rcn], w=[oan])
                    del hd[h]
            if "OAd" in self.dbg:
                if G == 0:
                    self.oad = self.dscr("OAd", [S, D], BF16)
                X.dma("sp", self.oad[G * 512:(G + 1) * 512, :].rearrange("(q p) d -> p q d", p=128), oa[:], r=[oan],
                      w=["OAd"], key="d_oad")
            tsl = slice(G * 512, (G + 1) * 512)
            sg_, sgn = sga.get()
            X.dma("sp", sg_[:], fmv(Sx["SGA"])[:, :, tsl], r=["SGA"], w=[sgn])
            ob_, obn = obg.get()
            X.dma("sp", ob_[:], fmv(Sx["OBG"])[:, :, tsl], r=["OBG"], w=[obn])
            x_, xn_ = xg.get()
            X.dma("sp", x_[:], fmv(Sx["XT"])[:, :, tsl], r=["XT"], w=[xn_])
            y_, yn_ = yT.get()
            for fc in range(KC):
                tr, trn = pTr.get()
                for qb in range(4):
                    X.s.add("pe", (lambda tr, qb, fc, oa: lambda e: e.transpose(
                        tr[:, qb * 128:(qb + 1) * 128], oa[:, qb, fc * 128:(fc + 1) * 128], T["ident_b"][:]))(tr, qb, fc, oa),
                        r=[oan], w=[trn])
                m_, mn_ = mt.get()
                X.tt("dve", m_[:], tr[:], sg_[:, fc, :], ALU.mult, r=[trn, sgn], w=[mn_])
                X.tt("pool", y_[:, fc, :], m_[:], ob_[:, fc, :], ALU.add, r=[mn_, obn], w=[yn_])
            for dc in range(KC):
                py, pyn = pY.get()
                for fc in range(KC):
                    X.mm(py[:], WO[:, fc, dc * 128:(dc + 1) * 128], y_[:, fc, :], fc == 0, fc == KC - 1,
                         r=["WO", yn_], w=[pyn])
                X.stt("dve", x_[:, dc, :], py[:], T["ADA"][:, l, g1c + dc:g1c + dc + 1], x_[:, dc, :], ALU.mult, ALU.add,
                      r=[pyn, xn_], w=[xn_])
            X.dma("sp", fmv(Sx["XT"])[:, :, tsl], x_[:], r=[xn_], w=["XT"], key="d_st" + xn_)

    def phase_M(self, s, sb, ps):
        I, T, Sx = self.I, self.T, self.Sx
        l = self.l
        X = Ops(s)
        moe = (l % 2 == 1)
        li = l // 2
        NFC = 28 if moe else 22
        ne = NE if moe else 1
        last = (l == self.n_layers - 1)
        fmv = lambda ap: ap.rearrange("(j p) t -> p j t", p=128)
        hT = sb("hT2", [128, KC, 1024], BF16)
        xa = sb("xa", [128, KC, 1024], F32)
        hid = sb("hid", [128, NFC, 1024], BF16)
        sqr = Rot(sb, "msq", 2, [128, 512], F32)
        nt = Rot(sb, "mnt", 2, [128, 512], F32)
        rstd = Rot(sb, "mrs", 1, [128, 512], F32)
        pss = Rot(ps, "mpss", 1, [128, 512], F32)
        pG = Rot(ps, "pG", 2, [128, 512], F32)
        pUu = Rot(ps, "pUu", 2, [128, 512], F32)
        pD = Rot(ps, "pD", 2, [128, 512], F32)
        wg = Rot(sb, "wg", 2, [128, KC, 128], BF16)
        wu = Rot(sb, "wu", 2, [128, KC, 128], BF16)
        wd = Rot(sb, "wd", 2, [128, NFC, 128], BF16)
        wgs = Rot(sb, "wgs", 2, [128, KC, 128], F32)
        wus = Rot(sb, "wus", 2, [128, KC, 128], F32)
        NH = NFC // 2
        wds = Rot(sb, "wds", 2, [128, NH, 128], F32)
        sgt = Rot(sb, "sgt", 2, [128, 512], F32)
        tmpd = Rot(sb, "tmpd", 2, [128, 512], F32)
        if moe:
            wr = sb("wr", [128, KC, NE], F32)
            X.dma("sp", wr[:], I["w_router"][li], r=[], w=["wr"])
            h32 = Rot(sb, "h32", 1, [128, 512], F32)
            pL = Rot(ps, "pL", 1, [128, 512], F32)
            CT = sb("CT", [8, 1024], F32)
            CBr = Rot(sb, "CBe", 2, [128, 1024], BF16)
            lg = sb("lg", [128, 8, NE], F32)
            small = {nm: sb("sm_" + nm, [128, 8, NE], F32) for nm in ["eq", "l2", "sel", "ex", "w"]}
            m1 = sb("m1", [128, 8], F32)
            m2 = sb("m2", [128, 8], F32)
            ssum = sb("ssum", [128, 8], F32)
        A2, Bc, g2c = T["A2"], 24, 40
        for SG in range(4):
            tsl = slice(SG * 1024, (SG + 1) * 1024)
            X.dma("sp", xa[:], fmv(Sx["XT"])[:, :, tsl], r=["XT"], w=["xa"])
            if moe:
                pl, pln = pL.get()
            for hf in range(2):
                hs = slice(hf * 512, (hf + 1) * 512)
                p_, pn_ = pss.get()
                for kc in range(KC):
                    q_, qn_ = sqr.get()
                    X.tt("pool", q_[:], xa[:, kc, hs], xa[:, kc, hs], ALU.mult, r=["xa"], w=[qn_])
                    X.mm(p_[:], T["ones_f"][:, 0:128], q_[:], kc == 0, kc == KC - 1, r=[qn_], w=[pn_])
                r_, rn_ = rstd.get()
                X.act(r_[:], p_[:], AF.Ln, r=[pn_], w=[rn_], bias=self.cbias(float(D * EPS)))
                X.act(r_[:], r_[:], AF.Exp, r=[rn_], w=[rn_], scale=-0.5)
                for kc in range(KC):
                    t_, tn_ = nt.get()
                    X.tt("dve", t_[:], xa[:, kc, hs], r_[:], ALU.mult, r=["xa", rn_], w=[tn_])
                    X.act(hT[:, kc, hs], t_[:], AF.Identity, r=[tn_], w=["hT2_%d" % hf],
                          bias=T["ADA"][:, l, Bc + kc:Bc + kc + 1], scale=A2[:, l, kc:kc + 1])
                    if moe:
                        h_, hn_ = h32.get()
                        X.ts("dve", h_[:], t_[:], A2[:, l, kc:kc + 1], T["ADA"][:, l, Bc + kc:Bc + kc + 1], ALU.mult, ALU.add,
                             r=[tn_], w=[hn_])
                        for tb in range(4):
                            col = (hf * 4 + tb) * NE
                            X.mm(pl[:, col:col + NE], h_[:, tb * 128:(tb + 1) * 128], wr[:, kc, :],
                                 hf == 0 and kc == 0 and tb == 0, kc == KC - 1, r=[hn_, "wr"], w=[pln], skip=True)
            if moe:
                v3 = lambda t: t[:]
                bc = lambda t: t[:].unsqueeze(2).broadcast_to([128, 8, NE])
                X.tt("dve", lg[:], pl[:, 0:64].rearrange("p (b e) -> p b e", e=NE),
                     T["brout"][:, li:li + 1, :].broadcast_to([128, 8, NE]), ALU.add, r=[pln], w=["lg"])
                X.s.add("dve", lambda e: e.tensor_reduce(out=m1[:], in_=lg[:], axis=AX.X, op=ALU.max), r=["lg"], w=["m1"])
                X.tt("dve", small["eq"][:], lg[:], bc(m1), ALU.is_equal, r=["lg", "m1"], w=["eq"])
                X.stt("dve", small["l2"][:], small["eq"][:], -1e30, lg[:], ALU.mult, ALU.add, r=["eq", "lg"], w=["l2"])
                X.s.add("dve", lambda e: e.tensor_reduce(out=m2[:], in_=small["l2"][:], axis=AX.X, op=ALU.max), r=["l2"], w=["m2"])
                X.tt("dve", small["sel"][:], lg[:], bc(m2), ALU.is_ge, r=["lg", "m2"], w=["sel"])
                X.tt("dve", small["ex"][:], lg[:], bc(m1), ALU.subtract, r=["lg", "m1"], w=["ex"])
                X.act(small["ex"][:], small["ex"][:], AF.Exp, r=["ex"], w=["ex"])
                X.tt("dve", small["w"][:], small["ex"][:], small["sel"][:], ALU.mult, r=["ex", "sel"], w=["w"])
                X.s.add("dve", lambda e: e.tensor_reduce(out=ssum[:], in_=small["w"][:], axis=AX.X, op=ALU.add), r=["w"], w=["ssum"])
                X.s.add("dve", lambda e: e.reciprocal(ssum[:], ssum[:]), r=["ssum"], w=["ssum"])
                X.tt("dve", small["w"][:], small["w"][:], bc(ssum), ALU.mult, r=["w", "ssum"], w=["w"])
                for hf in range(2):
                    p_, pn_ = pD.get()
                    for tb in range(4):
                        X.s.add("pe", (lambda p_, tb, hf: lambda e: e.transpose(
                            p_[0:8, tb * 128:(tb + 1) * 128], small["w"][:, hf * 4 + tb, :], T["ident_f"][:]))(p_, tb, hf),
                            r=["w"], w=[pn_])
                    X.copy("dve", CT[:, hf * 512:(hf + 1) * 512], p_[0:8, :], r=[pn_], w=["CT"])
            for e_ in range(ne):
                if moe:
                    CB, cbn = CBr.get()
                    for hf in range(2):
                        p_, pn_ = pD.get()
                        X.mm(p_[:], T["sel8"][:, e_, :], CT[:, hf * 512:(hf + 1) * 512], True, True, r=["CT"], w=[pn_])
                        X.copy("dve", CB[:, hf * 512:(hf + 1) * 512], p_[:], r=[pn_], w=[cbn])
                for fcn in range(NFC):
                    g32, g32n = wgs.get()
                    u32, u32n = wus.get()
                    X.dma("sp", g32[:], (I["mwg"][li, e_, fcn] if moe else I["dwg"][li, fcn]), r=[], w=[g32n])
                    X.dma("sp", u32[:], (I["mwu"][li, e_, fcn] if moe else I["dwu"][li, fcn]), r=[], w=[u32n])
                    g_, gn_ = wg.get()
                    u_, un_ = wu.get()
                    X.copy("pool", g_[:], g32[:], r=[g32n], w=[gn_])
                    X.copy("pool", u_[:], u32[:], r=[u32n], w=[un_])
                    for hf in range(2):
                        hs = slice(hf * 512, (hf + 1) * 512)
                        pg, pgn = pG.get()
                        for kc in range(KC):
                            X.mm(pg[:], g_[:, kc, :], hT[:, kc, hs], kc == 0, kc == KC - 1, r=[gn_, "hT2_%d" % hf], w=[pgn])
                        pu, pun = pUu.get()
                        for kc in range(KC):
                            X.mm(pu[:], u_[:, kc, :], hT[:, kc, hs], kc == 0, kc == KC - 1, r=[un_, "hT2_%d" % hf], w=[pun])
                        sg_, sgn = sgt.get()
                        X.act(sg_[:], pg[:], AF.Silu, r=[pgn], w=[sgn])
                        X.tt("dve", hid[:, fcn, hs], pu[:], sg_[:], ALU.mult, r=[pun, sgn], w=["hid"])
                for dc in range(KC):
                    d_, dn_ = wd.get()
                    src = (I["mwd"][li, e_][:, :, dc * 128:(dc + 1) * 128] if moe else I["dwd"][li][:, :, dc * 128:(dc + 1) * 128])
                    for hh in range(2):
                        d32, d32n = wds.get()
                        X.dma("sp", d32[:], src[:, hh * NH:(hh + 1) * NH, :], r=[], w=[d32n])
                        X.copy("pool", d_[:, hh * NH:(hh + 1) * NH, :], d32[:], r=[d32n], w=[dn_])
                    for hf in range(2):
                        hs = slice(hf * 512, (hf + 1) * 512)
                        pd, pdn = pD.get()
                        for fcn in range(NFC):
                            X.mm(pd[:], d_[:, fcn, :], hid[:, fcn, hs], fcn == 0, fcn == NFC - 1, r=[dn_, "hid"], w=[pdn])
                        gcol = T["ADA"][:, l, g2c + dc:g2c + dc + 1]
                        if moe:
                            t_, tn_ = tmpd.get()
                            X.stt("dve", t_[:], pd[:], gcol, CB[:, hs], ALU.mult, ALU.mult, r=[pdn, cbn], w=[tn_])
                            X.tt("pool", xa[:, dc, hs], xa[:, dc, hs], t_[:], ALU.add, r=[tn_, "xa"], w=["xa"])
                        else:
                            X.stt("dve", xa[:, dc, hs], pd[:], gcol, xa[:, dc, hs], ALU.mult, ALU.add, r=[pdn, "xa"], w=["xa"])
            dst = self.out if last else Sx["XT"]
            X.dma("sp", fmv(dst)[:, :, tsl], xa[:], r=["xa"], w=["XT"], key="d_stxa")


class Rot:
    def __init__(self, alloc, name, n, shape, dt):
        self.t = [(alloc("%s%d" % (name, i), shape, dt), "%s%d" % (name, i)) for i in range(n)]
        self.i = 0

    def get(self):
        t = self.t[self.i % len(self.t)]
        self.i += 1
        return t


class Ops:
    def __init__(self, s):
        self.s = s

    def mm(self, out, lhsT, rhs, start, stop, r, w, skip=False):
        if skip:
            self.s.add("pe", lambda e: e.matmul(out, lhsT, rhs, start=start, stop=stop, skip_group_check=True), r=r, w=w)
        else:
            self.s.add("pe", lambda e: e.matmul(out, lhsT, rhs, start=start, stop=stop), r=r, w=w)

    def act(self, out, in_, func, r, w, bias=None, scale=None):
        kw = {}
        if bias is not None:
            kw["bias"] = bias
        if scale is not None:
            kw["scale"] = scale
        self.s.add("act", lambda e: e.activation(out=out, in_=in_, func=func, **kw), r=r, w=w)

    def tt(self, eng, out, in0, in1, op, r, w):
        self.s.add(eng, lambda e: e.tensor_tensor(out=out, in0=in0, in1=in1, op=op), r=r, w=w)

    def ts(self, eng, out, in0, s1, s2, op0, op1, r, w):
        self.s.add(eng, lambda e: e.tensor_scalar(out=out, in0=in0, scalar1=s1, scalar2=s2, op0=op0, op1=op1), r=r, w=w)

    def stt(self, eng, out, in0, scalar, in1, op0, op1, r, w):
        self.s.add(eng, lambda e: e.scalar_tensor_tensor(out=out, in0=in0, scalar=scalar, in1=in1, op0=op0, op1=op1),
                   r=r, w=w)

    def copy(self, eng, out, in_, r, w):
        self.s.add(eng, lambda e: e.tensor_copy(out, in_), r=r, w=w)

    def memset(self, eng, ap, v, w):
        self.s.add(eng, lambda e: e.memset(ap, v), w=w)

    def dma(self, eng, out, in_, r, w, key=None):
        if key is None:
            key = "d_" + w[0]
        self.s.add(eng, lambda e: e.dma_start(out=out, in_=in_), r=r, w=w, sem=key)


def _consts():
    ident = np.eye(128, dtype=np.float32)
    bd64 = np.zeros((128, 128), np.float32)
    bd64[:64, :64] = 1.0
    bd64[64:, 64:] = 1.0
    ss, tt = np.meshgrid(np.arange(64), np.arange(64), indexing="ij")
    tri64 = (ss <= tt).astype(np.float32)
    maskneg = np.zeros((128, 512), np.float32)
    ss, tt = np.meshgrid(np.arange(128), np.arange(128), indexing="ij")
    maskneg[:, :128] = np.where(ss > tt, -30000.0, 0.0)
    resetm = np.ones((128, 512), np.float32)
    resetm[:, ::CH] = 0.0
    sel8 = np.zeros((8, NE, 128), np.float32)
    for e in range(NE):
        sel8[e, e, :] = 1.0
    return dict(ident=ident, bd64=bd64, tri64=tri64, maskneg=maskneg, resetm=resetm, sel8=sel8)


def _pf(a):
    a = np.asarray(a, np.float32)
    lead = a.shape[:-1]
    n = a.shape[-1] // 128
    a = a.reshape(lead + (n, 128))
    return np.ascontiguousarray(np.moveaxis(a, -1, 0))


def _wtiles(w, ncol):
    K, N = w.shape
    kc = K // 128
    return np.ascontiguousarray(w.reshape(kc, 128, N // ncol, ncol).transpose(2, 1, 0, 3))


def _layout_fns(inp):
    f = lambda k: np.asarray(inp[k], np.float32)
    o = {}
    o["w_ada"] = lambda: np.stack([_wtiles(f("w_ada")[l], 768) for l in range(DEPTH)])
    o["b_ada"] = lambda: np.ascontiguousarray(_pf(f("b_ada")))
    o["norm_mix"] = lambda: _pf(f("norm_mix"))
    o["norm_ffn"] = lambda: _pf(f("norm_ffn"))

    def w_in_groups():
        w_in = f("w_in")
        offs = np.cumsum([0, 1024, 1024, 1024, 16, 1024, 1024, 1024, 1024, 1024, 1024])
        fq, fk, fv, ff, hq, hf, hi, hg, ga, gb = [w_in[:, :, offs[i]:offs[i + 1]] for i in range(10)]
        groups = [fq[:, :, :512], fq[:, :, 512:], fk[:, :, :512], fk[:, :, 512:], fv[:, :, :512], fv[:, :, 512:],
                  hi[:, :, :512], hi[:, :, 512:]]
        sl = lambda a, j: a[:, :, j * 128:(j + 1) * 128]
        for j in range(0, 8, 2):
            groups.append(np.concatenate([sl(hf, j), sl(hq, j), sl(hf, j + 1), sl(hq, j + 1)], axis=2))
        for j in range(0, 8, 2):
            groups.append(np.concatenate([sl(hg, j), sl(gb, j), sl(hg, j + 1), sl(gb, j + 1)], axis=2))
        groups += [ga[:, :, :512], ga[:, :, 512:]]
        wr = np.concatenate(groups, axis=2)
        return np.stack([_wtiles(wr[l], 512) for l in range(DEPTH)])

    o["w_in"] = w_in_groups
    o["w_ff"] = lambda: np.ascontiguousarray(f("w_in")[:, :, 3072:3088].reshape(DEPTH, KC, 128, 16).transpose(0, 2, 1, 3))
    o["qgain"] = lambda: np.ascontiguousarray(np.tile(f("fox_q_gain"), (1, 2)).T)
    o["kgain"] = lambda: np.ascontiguousarray(np.tile(f("fox_k_gain"), (1, 2)).T)
    o["fbias"] = lambda: np.ascontiguousarray(np.broadcast_to(f("fox_f_bias")[None], (128, DEPTH, 16)))
    o["hg_lb"] = lambda: np.ascontiguousarray(_pf(f("hg_lb")).transpose(0, 2, 1))
    o["ogain"] = lambda: np.ascontiguousarray(f("hg_o_gain").T)
    o["w_out"] = lambda: np.ascontiguousarray(f("w_out").reshape(DEPTH, KC, 128, D).transpose(0, 2, 1, 3))
    o["w_router"] = lambda: np.ascontiguousarray(f("w_router").reshape(2, KC, 128, NE).transpose(0, 2, 1, 3))
    o["b_router"] = lambda: np.ascontiguousarray(np.broadcast_to(f("b_router")[None], (128, 2, NE)))
    o["dwg"] = lambda: np.stack([_wtiles(f("dense_w_gate")[i], 128) for i in range(2)])
    o["dwu"] = lambda: np.stack([_wtiles(f("dense_w_up")[i], 128) for i in range(2)])
    o["dwd"] = lambda: np.ascontiguousarray(f("dense_w_down").reshape(2, 22, 128, D).transpose(0, 2, 1, 3))
    o["mwg"] = lambda: np.stack([np.stack([_wtiles(f("moe_w_gate")[i, e], 128) for e in range(NE)]) for i in range(2)])
    o["mwu"] = lambda: np.stack([np.stack([_wtiles(f("moe_w_up")[i, e], 128) for e in range(NE)]) for i in range(2)])
    o["mwd"] = lambda: np.ascontiguousarray(f("moe_w_down").reshape(2, NE, 28, 128, D).transpose(0, 1, 3, 2, 4))
    for k, v in _consts().items():
        o[k] = (lambda v: lambda: v)(v)
    return o


def _layout_shared_subset(inp, keys):
    fns = _layout_fns(inp)
    return {k: fns[k]() for k in keys if k in fns}


def _layout_shared(inp):
    fns = _layout_fns(inp)
    return {k: fn() for k, fn in fns.items()}


def make_in_maps(inp, n_cores=8):
    shared = _layout_shared(inp)
    x = np.asarray(inp["x"], np.float32)
    c = np.asarray(inp["c"], np.float32)
    maps = []
    for b in range(n_cores):
        m = dict(shared)
        m["xT"] = np.ascontiguousarray(x[b].T)
        m["c"] = np.ascontiguousarray(c[b].reshape(KC, 128).T)
        maps.append(m)
    return maps


def kernel(**inp):
    mk = MK()
    nc = mk.build()
    maps = make_in_maps(inp)
    res = run_bass_kernel_spmd(nc, maps, core_ids=list(range(8)))
    out = np.stack([np.ascontiguousarray(r["yT"].T) for r in res.results], axis=0)
    return out.astype(np.float32)
```

```python
import numpy as np
from contextlib import ExitStack
import concourse.bass as bass
import concourse.mybir as mybir
from concourse.bass_utils import run_bass_kernel_spmd

F32 = mybir.dt.float32
BF16 = mybir.dt.bfloat16
AF = mybir.ActivationFunctionType
ALU = mybir.AluOpType
AX = mybir.AxisListType

S = 4096
D = 1024
KC = 8
NG = 8
NB = 32
DEPTH = 4
EPS = 1e-6
DFF = 2816
DFE = 3584
NE = 8
CH = 32
NCS = 512 // CH


class _Op:
    __slots__ = ("eng", "fn", "sem", "inc", "deps", "needed", "sigval", "idx")


class Sched:
    EPOCH = 12000

    def __init__(self, nc):
        self.nc = nc
        self.ops = []
        self.last_w = {}
        self.readers = {}
        self.neng = {}

    def add(self, eng, fn, r=(), w=(), sem=None):
        op = _Op()
        op.eng = eng
        op.fn = fn
        if sem is None:
            n = self.neng.get(eng, 0)
            self.neng[eng] = n + 1
            op.sem = "%s#%d" % (eng, n // self.EPOCH)
            op.inc = 1
            op.needed = False
        else:
            op.sem = sem
            op.inc = 16
            op.needed = True
        deps = {}

        def dep(o):
            if o is None:
                return
            if o.eng == "pe" and eng == "pe":
                return
            k = o.sem
            if k not in deps or deps[k].idx < o.idx:
                deps[k] = o

        for x in r:
            dep(self.last_w.get(x))
        for x in w:
            dep(self.last_w.get(x))
            rd = self.readers.get(x)
            if rd:
                for o in rd.values():
                    dep(o)
        op.idx = len(self.ops)
        op.deps = deps
        for o in deps.values():
            o.needed = True
        for x in r:
            self.readers.setdefault(x, {})[op.sem] = op
        for x in w:
            self.last_w[x] = op
            self.readers[x] = {}
        self.ops.append(op)
        return op

    def emit(self):
        nc = self.nc
        last = {}
        for op in self.ops:
            if op.fn is not None:
                last[op.sem] = op
        for op in last.values():
            op.needed = True
        cnt = {}
        for op in self.ops:
            if op.fn is not None and op.needed:
                cnt[op.sem] = cnt.get(op.sem, 0) + op.inc
                op.sigval = cnt[op.sem]
        keys = sorted(cnt.keys())
        sems = {k: nc.alloc_semaphore(name="s_" + k.replace("#", "_") + getattr(self, "uid", "")) for k in keys}
        with nc.Block() as block:

            def run(engname):
                def f(e):
                    waited = {}
                    for op in self.ops:
                        if op.eng != engname:
                            continue
                        for k, d in op.deps.items():
                            if waited.get(k, 0) >= d.sigval:
                                continue
                            e.wait_ge(sems[k], d.sigval)
                            waited[k] = d.sigval
                        if op.fn is not None:
                            ins = op.fn(e)
                            if op.needed:
                                ins.then_inc(sems[op.sem], op.inc)
                    for k in keys:
                        if waited.get(k, 0) < cnt[k]:
                            e.wait_ge(sems[k], cnt[k])
                return f

            block.tensor(run("pe"))
            block.scalar(run("act"))
            block.vector(run("dve"))
            block.gpsimd(run("pool"))
            block.sync(run("sp"))
        nc.all_engine_barrier()
        nc.clear_and_free_semaphores(list(sems.values()))
        nc.all_engine_barrier()


class MK:
    def __init__(self, n_layers=DEPTH, dbg=(), stop_after=None):
        self.n_layers = n_layers
        self.dbg = set(dbg)
        self.stop_after = stop_after
        self.nc = bass.Bass("TRN2", target_bir_lowering=False)
        self.es = ExitStack()
        self.uid = 0

    def din(self, name, shape, dt=F32):
        return self.nc.dram_tensor(name, list(shape), dt, kind="ExternalInput").ap()

    def dscr(self, name, shape, dt):
        kind = "ExternalOutput" if name in self.dbg else "Internal"
        return self.nc.dram_tensor(name, list(shape), dt, kind=kind).ap()

    def persist(self, name, shape, dt):
        return self.es.enter_context(self.nc.sbuf_tensor(name, list(shape), dt))

    CB = [float(D * EPS), float(64 * EPS), float(128 * EPS)]

    def cbias(self, v):
        i = self.CB.index(v)
        return self.T["cb"][:, i:i + 1]

    def phase(self, body):
        nc = self.nc
        with ExitStack() as es:
            s = Sched(nc)
            self.uid += 1
            u = "_u%d" % self.uid
            s.uid = u

            def sb(name, shape, dt=F32):
                return es.enter_context(nc.sbuf_tensor(name + u, list(shape), dt))

            def ps(name, shape, dt=F32):
                return es.enter_context(nc.psum_tensor(name + u, list(shape), dt))

            body(s, sb, ps)
            s.emit()

    def build(self):
        nc = self.nc
        L = self.n_layers
        shapes = {
            "xT": [D, S], "c": [128, KC], "w_ada": [DEPTH, 8, 128, KC, 768], "b_ada": [128, DEPTH, 48],
            "norm_mix": [128, DEPTH, KC], "norm_ffn": [128, DEPTH, KC], "w_in": [DEPTH, 18, 128, KC, 512],
            "w_ff": [DEPTH, 128, KC, 16], "qgain": [128, DEPTH], "kgain": [128, DEPTH], "fbias": [128, DEPTH, 16],
            "hg_lb": [128, KC, DEPTH], "ogain": [128, DEPTH], "w_out": [DEPTH, 128, KC, D],
            "w_router": [2, 128, KC, NE], "b_router": [128, 2, NE],
            "dwg": [2, 22, 128, KC, 128], "dwu": [2, 22, 128, KC, 128], "dwd": [2, 128, 22, D],
            "mwg": [2, NE, 28, 128, KC, 128], "mwu": [2, NE, 28, 128, KC, 128], "mwd": [2, NE, 128, 28, D],
            "ident": [128, 128], "bd64": [128, 128], "tri64": [64, 64], "maskneg": [128, 512],
            "resetm": [128, 512], "sel8": [8, NE, 128],
        }
        mk = self

        class _Lazy(dict):
            def __missing__(self, k):
                v = mk.din(k, shapes[k])
                self[k] = v
                return v

        I = _Lazy()
        self.I = I
        self.out = self.nc.dram_tensor("yT", [D, S], F32, kind="ExternalOutput").ap()

        Sx = {}
        Sx["XT"] = self.dscr("XT", [D, S], F32)
        Sx["QT"] = self.dscr("QT", [16, 65, S], BF16)
        Sx["KT"] = self.dscr("KT", [16, 65, S], BF16)
        Sx["V"] = self.dscr("V", [S, 16 * 65], BF16)
        Sx["QM"] = self.dscr("QM", [D, S], BF16)
        Sx["KM"] = self.dscr("KM", [D, S], BF16)
        Sx["QI"] = self.dscr("QI", [D, S], BF16)
        Sx["KL"] = self.dscr("KL", [D, S], BF16)
        Sx["VH"] = self.dscr("VH", [S, D], BF16)
        Sx["GG"] = self.dscr("GG", [D, S], BF16)
        Sx["SGA"] = self.dscr("SGA", [D, S], BF16)
        Sx["OBG"] = self.dscr("OBG", [D, S], BF16)
        self.Sx = Sx

        T = {}
        T["ident_f"] = self.persist("ident_f", [128, 128], F32)
        T["ident_b"] = self.persist("ident_b", [128, 128], BF16)
        T["ones_f"] = self.persist("ones_f", [128, 512], F32)
        T["bd64"] = self.persist("bd64_b", [128, 128], BF16)
        T["tri64"] = self.persist("tri64_f", [64, 64], F32)
        T["maskneg"] = self.persist("maskneg_b", [128, 512], BF16)
        T["resetm"] = self.persist("resetm_f", [128, 512], F32)
        T["sel8"] = self.persist("sel8_f", [8, NE, 128], F32)
        T["ADA"] = self.persist("ADA", [128, DEPTH, 48], F32)
        T["A1"] = self.persist("A1", [128, DEPTH, KC], F32)
        T["A2"] = self.persist("A2", [128, DEPTH, KC], F32)
        T["LB"] = self.persist("LB", [128, KC, DEPTH], F32)
        T["OML"] = self.persist("OML", [128, KC, DEPTH], F32)
        T["NOML"] = self.persist("NOML", [128, KC, DEPTH], F32)
        T["qgain"] = self.persist("qgain_s", [128, DEPTH], F32)
        T["kgain"] = self.persist("kgain_s", [128, DEPTH], F32)
        T["ogain"] = self.persist("ogain_s", [128, DEPTH], F32)
        T["kgain8"] = self.persist("kgain8", [128, DEPTH], F32)
        T["ogainS"] = self.persist("ogainS", [128, DEPTH], F32)
        T["fbias"] = self.persist("fbias_s", [128, DEPTH, 16], F32)
        T["brout"] = self.persist("brout_s", [128, 2, NE], F32)
        T["EGL"] = self.persist("EGL", [128, KC, S // CH], F32)
        T["cb"] = self.persist("cbias", [128, 8], F32)
        T["LF"] = self.persist("LF", [128, NB, 16], F32)
        T["NF"] = self.persist("NF", [128, NB, 16], F32)
        self.T = T

        self.phase(self.prologue)
        if self.stop_after == "prologue":
            return self.finish()
        if isinstance(self.stop_after, tuple) and self.stop_after[0] == "onlyM":
            self.l = self.stop_after[1]
            self.n_layers = self.l + 1
            self.phase(self.phase_M)
            return self.finish()
        for l in range(L):
            self.l = l
            self.phase(self.phase_A)
            if self.stop_after in (("A", l), ("A1", l)):
                return self.finish()
            self.phase(self.phase_H)
            if self.stop_after == ("H", l):
                return self.finish()
            self.phase(self.phase_T)
            if self.stop_after == ("T", l):
                return self.finish()
            self.phase(self.phase_M)
            if self.stop_after == ("M", l):
                return self.finish()
        return self.finish()

    def finish(self):
        self.es.close()
        return self.nc

    def prologue(self, s, sb, ps):
        I, T, Sx = self.I, self.T, self.Sx
        stage = sb("pstage", [128, 512], F32)
        stage2 = sb("pstage2", [128, 128], F32)
        s.add("sp", lambda e: e.dma_start(out=T["ident_f"][:], in_=I["ident"]), w=["ident_f"], sem="d0_1")
        s.add("sp", lambda e: e.dma_start(out=stage2[:], in_=I["bd64"]), w=["stage2"], sem="d0_2")
        s.add("sp", lambda e: e.dma_start(out=T["tri64"][:], in_=I["tri64"]), w=["tri64"], sem="d0_3")
        s.add("sp", lambda e: e.dma_start(out=stage[:], in_=I["maskneg"]), w=["stage"], sem="d0_4")
        s.add("sp", lambda e: e.dma_start(out=T["resetm"][:], in_=I["resetm"]), w=["resetm"], sem="d0_5")
        s.add("sp", lambda e: e.dma_start(out=T["sel8"][:], in_=I["sel8"]), w=["sel8"], sem="d0_6")
        s.add("dve", lambda e: e.tensor_copy(T["ident_b"][:], T["ident_f"][:]), r=["ident_f"], w=["ident_b"])
        s.add("dve", lambda e: e.tensor_copy(T["bd64"][:], stage2[:]), r=["stage2"], w=["bd64"])
        s.add("dve", lambda e: e.tensor_copy(T["maskneg"][:], stage[:]), r=["stage"], w=["maskneg"])
        s.add("pool", lambda e: e.memset(T["ones_f"][:], 1.0), w=["ones_f"])
        for i, v in enumerate(self.CB):
            s.add("pool", (lambda i, v: lambda e: e.memset(T["cb"][:, i:i + 1], v))(i, v), w=["cb"])
        for nm in ["qgain", "kgain", "ogain", "fbias"]:
            s.add("sp", (lambda nm: lambda e: e.dma_start(out=T[nm][:], in_=I[nm]))(nm), w=[nm], sem="d0_" + nm)
        s.add("sp", lambda e: e.dma_start(out=T["brout"][:], in_=I["b_router"]), w=["brout"], sem="d0_8")
        s.add("dve", lambda e: e.tensor_scalar(out=T["kgain8"][:], in0=T["kgain"][:], scalar1=8.0, scalar2=None,
                                               op0=ALU.mult), r=["kgain"], w=["kgain8"])
        s.add("dve", lambda e: e.tensor_scalar(out=T["ogainS"][:], in0=T["ogain"][:], scalar1=float(128 ** 0.5),
                                               scalar2=None, op0=ALU.mult), r=["ogain"], w=["ogainS"])
        nmix = sb("nmix", [128, DEPTH, KC], F32)
        nffn = sb("nffn", [128, DEPTH, KC], F32)
        bada = sb("bada", [128, DEPTH, 48], F32)
        cin = sb("cin", [128, KC], F32)
        cact = sb("cact", [128, KC], F32)
        lbin = sb("lbin", [128, KC, DEPTH], F32)
        s.add("sp", lambda e: e.dma_start(out=nmix[:], in_=I["norm_mix"]), w=["nmix"], sem="d0_9")
        s.add("sp", lambda e: e.dma_start(out=nffn[:], in_=I["norm_ffn"]), w=["nffn"], sem="d0_10")
        s.add("sp", lambda e: e.dma_start(out=bada[:], in_=I["b_ada"]), w=["bada"], sem="d0_11")
        s.add("sp", lambda e: e.dma_start(out=cin[:], in_=I["c"]), w=["cin"], sem="d0_12")
        s.add("sp", lambda e: e.dma_start(out=lbin[:], in_=I["hg_lb"]), w=["lbin"], sem="d0_13")
        s.add("sp", lambda e: e.dma_start(out=Sx["XT"], in_=I["xT"]), w=["XT"], sem="d1")
        onesb = sb("onesb", [16, S], BF16)
        s.add("pool", lambda e: e.memset(onesb[:], 1.0), w=["onesb"])
        s.add("sp", lambda e: e.dma_start(out=Sx["KT"][:, 64, :], in_=onesb[:]), r=["onesb"], w=["KT64"], sem="d1b")
        s.add("act", lambda e: e.activation(out=cact[:], in_=cin[:], func=AF.Silu), r=["cin"], w=["cact"])
        wa = [sb("wa%d" % i, [128, KC, 768], F32) for i in range(2)]
        pada = ps("pada", [128, 512], F32)
        n = 0
        for l in range(DEPTH):
            for blk in range(8):
                b = n % 2
                n += 1
                s.add("sp", (lambda b, l, blk: lambda e: e.dma_start(out=wa[b][:], in_=I["w_ada"][l, blk]))(b, l, blk),
                      w=["wa%d" % b], sem="dwa%d" % b)
                for m in range(6):
                    col = l * 48 + blk * 6 + m
                    for kc in range(KC):
                        s.add("pe", (lambda b, m, kc, col: lambda e: e.matmul(
                            pada[:, col:col + 1], wa[b][:, kc, m * 128:(m + 1) * 128], cact[:, kc:kc + 1],
                            start=(kc == 0), stop=(kc == KC - 1)))(b, m, kc, col),
                            r=["wa%d" % b, "cact"], w=["pada"])
        s.add("dve", lambda e: e.tensor_tensor(
            out=T["ADA"][:].rearrange("p l m -> p (l m)"), in0=pada[:, 0:DEPTH * 48],
            in1=bada[:].rearrange("p l m -> p (l m)"), op=ALU.add), r=["pada", "bada"], w=["ADA"])
        tmpA = sb("tmpA", [128, DEPTH, KC], F32)
        s.add("dve", lambda e: e.tensor_scalar(out=tmpA[:], in0=T["ADA"][:, :, 8:16], scalar1=1.0, scalar2=32.0,
                                               op0=ALU.add, op1=ALU.mult), r=["ADA"], w=["tmpA"])
        s.add("dve", lambda e: e.tensor_tensor(out=T["A1"][:], in0=tmpA[:], in1=nmix[:], op=ALU.mult),
              r=["tmpA", "nmix"], w=["A1"])
        tmpB = sb("tmpB", [128, DEPTH, KC], F32)
        s.add("dve", lambda e: e.tensor_scalar(out=tmpB[:], in0=T["ADA"][:, :, 32:40], scalar1=1.0, scalar2=32.0,
                                               op0=ALU.add, op1=ALU.mult), r=["ADA"], w=["tmpB"])
        s.add("dve", lambda e: e.tensor_tensor(out=T["A2"][:], in0=tmpB[:], in1=nffn[:], op=ALU.mult),
              r=["tmpB", "nffn"], w=["A2"])
        lbe = sb("lbe", [128, KC, DEPTH], F32)
        lbs = sb("lbs", [128, KC], F32)
        lbr = sb("lbr", [128, KC], F32)
        lbsm = sb("lbsm", [128, KC, DEPTH], F32)
        s.add("act", lambda e: e.activation(out=lbe[:], in_=lbin[:], func=AF.Exp), r=["lbin"], w=["lbe"])
        s.add("dve", lambda e: e.tensor_reduce(out=lbs[:], in_=lbe[:], axis=AX.X, op=ALU.add), r=["lbe"], w=["lbs"])
        s.add("dve", lambda e: e.reciprocal(lbr[:], lbs[:]), r=["lbs"], w=["lbr"])
        s.add("dve", lambda e: e.tensor_tensor(out=lbsm[:], in0=lbe[:],
                                               in1=lbr[:].unsqueeze(2).broadcast_to([128, KC, DEPTH]), op=ALU.mult),
              r=["lbe", "lbr"], w=["lbsm"])
        s.add("pool", lambda e: e.memset(T["LB"][:, :, 0:1], 0.0), w=["LB"])
        for l in range(1, DEPTH):
            s.add("dve", (lambda l: lambda e: e.tensor_tensor(
                out=T["LB"][:, :, l:l + 1], in0=T["LB"][:, :, l - 1:l], in1=lbsm[:, :, l:l + 1], op=ALU.add))(l),
                r=["LB", "lbsm"], w=["LB"])
        s.add("dve", lambda e: e.tensor_scalar(out=T["OML"][:], in0=T["LB"][:], scalar1=-1.0, scalar2=1.0,
                                               op0=ALU.mult, op1=ALU.add), r=["LB"], w=["OML"])
        s.add("dve", lambda e: e.tensor_scalar(out=T["NOML"][:], in0=T["LB"][:], scalar1=1.0, scalar2=-1.0,
                                               op0=ALU.mult, op1=ALU.add), r=["LB"], w=["NOML"])

    def norm_mod(self, s, sb, ps, hT, Atab, Aname, Bcol0, l, h32=None):
        T, Sx = self.T, self.Sx
        xg = [sb("xg%d" % i, [128, KC, 512], F32) for i in range(2)]
        sq = [sb("sq%d" % i, [128, 512], F32) for i in range(2)]
        tmp = [sb("nt%d" % i, [128, 512], F32) for i in range(2)]
        rstd = [sb("rstd%d" % i, [128, 512], F32) for i in range(2)]
        pss = [ps("pssq%d" % i, [128, 512], F32) for i in range(2)]
        xt = Sx["XT"].rearrange("(kc p) t -> p kc t", p=128)
        def do(g):
            b = g % 2
            s.add("sp", (lambda b, g: lambda e: e.dma_start(out=xg[b][:], in_=xt[:, :, g * 512:(g + 1) * 512]))(b, g),
                  r=["XT"], w=["xg%d" % b], sem="dxg%d" % b)
            for kc in range(KC):
                q = kc % 2
                s.add("pool", (lambda b, kc, q: lambda e: e.tensor_tensor(
                    out=sq[q][:], in0=xg[b][:, kc, :], in1=xg[b][:, kc, :], op=ALU.mult))(b, kc, q),
                    r=["xg%d" % b], w=["sq%d" % q])
                s.add("pe", (lambda b, kc, q: lambda e: e.matmul(
                    pss[b][:], T["ones_f"][:, 0:128], sq[q][:], start=(kc == 0), stop=(kc == KC - 1)))(b, kc, q),
                    r=["sq%d" % q, "ones_f"], w=["pss%d" % b])
            s.add("act", (lambda b: lambda e: e.activation(
                out=rstd[b][:], in_=pss[b][:], func=AF.Ln, bias=self.cbias(float(D * EPS))))(b),
                r=["pss%d" % b], w=["rstd%d" % b])
            s.add("act", (lambda b: lambda e: e.activation(
                out=rstd[b][:], in_=rstd[b][:], func=AF.Exp, scale=-0.5))(b),
                r=["rstd%d" % b], w=["rstd%d" % b])
            for kc in range(KC):
                q = kc % 2
                s.add("dve", (lambda b, kc, q: lambda e: e.tensor_tensor(
                    out=tmp[q][:], in0=xg[b][:, kc, :], in1=rstd[b][:], op=ALU.mult))(b, kc, q),
                    r=["xg%d" % b, "rstd%d" % b], w=["nt%d" % q])
                if h32 is None:
                    s.add("act", (lambda g, kc, q: lambda e: e.activation(
                        out=hT[:, kc, g * 512:(g + 1) * 512], in_=tmp[q][:], func=AF.Identity,
                        bias=T["ADA"][:, l, Bcol0 + kc:Bcol0 + kc + 1], scale=Atab[:, l, kc:kc + 1]))(g, kc, q),
                        r=["nt%d" % q, "ADA", Aname], w=["hT%d" % g])
        return xg, do

    def phase_A(self, s, sb, ps):
        I, T, Sx = self.I, self.T, self.Sx
        l = self.l
        hT = sb("hT", [128, KC, S], BF16)
        xg, norm_group = self.norm_mod(s, sb, ps, hT, T["A1"], "A1", 0, l)
        norm_group(0)
        if self.stop_after == ("A1", l):
            for g in range(1, NG):
                norm_group(g)
            dbg = self.dscr("HT", [D, S], BF16)
            s.add("sp", lambda e: e.dma_start(out=dbg.rearrange("(kc p) t -> p kc t", p=128), in_=hT[:]),
                  r=["hT%d" % g for g in range(NG)], w=["HTd"], sem="dbg")
            return
        X = Ops(s)
        wb = Rot(sb, "wb", 2, [128, KC, 512], BF16)
        pacc = Rot(ps, "pacc", 4, [128, 512], F32)
        pn = Rot(ps, "pn", 2, [128, 512], F32)
        stg = Rot(sb, "stg", 8, [128, 512], BF16)
        f32t = Rot(sb, "ft", 12, [128, 512], F32)
        sqb = Rot(sb, "sqb", 2, [128, 512], BF16)
        vst = Rot(sb, "vst", 2, [128, 8, 65], BF16)
        for t_, n_ in vst.t:
            X.memset("pool", t_[:], 1.0, w=[n_])
        wff = sb("wff", [128, KC, 16], BF16)
        X.dma("pool", wff[:], I["w_ff"][l], r=[], w=["wff"])
        tsl = lambda g: slice(g * 512, (g + 1) * 512)

        def mm_fm(w_, wn, ci, g):
            p_, pn_ = pacc.get()
            for kc in range(KC):
                X.mm(p_[:], w_[:, kc, ci * 128:(ci + 1) * 128], hT[:, kc, tsl(g)], kc == 0, kc == KC - 1,
                     r=[wn, "hT%d" % g], w=[pn_])
            return p_, pn_

        def rsq(src, srcn, cb):
            r_, rn = f32t.get()
            X.act(r_[:], src, AF.Ln, r=[srcn, "cb"], w=[rn], bias=self.cbias(cb))
            X.act(r_[:], r_[:], AF.Exp, r=[rn], w=[rn], scale=-0.5)
            return r_, rn

        for gi in range(18):
            w_, wn = wb.get()
            X.dma("pool", w_[:], I["w_in"][l, gi], r=[], w=[wn])
            if gi < 4:
                isq = gi < 2
                dst = Sx["QT"] if isq else Sx["KT"]
                gcol = T["qgain"][:, l:l + 1] if isq else T["kgain8"][:, l:l + 1]
                for g in range(NG):
                    if gi == 0 and g + 1 < NG:
                        norm_group(g + 1)
                    for ci in range(4):
                        fc = (gi % 2) * 4 + ci
                        p_, pn_ = mm_fm(w_, wn, ci, g)
                        q_, qn_ = sqb.get()
                        X.act(q_[:], p_[:], AF.Square, r=[pn_], w=[qn_])
                        n_, nn_ = pn.get()
                        X.mm(n_[:], T["bd64"][:], q_[:], True, True, r=[qn_, "bd64"], w=[nn_])
                        r_, rn = rsq(n_[:], nn_, float(64 * EPS))
                        o_, on_ = stg.get()
                        X.stt("dve", o_[:], p_[:], gcol, r_[:], ALU.mult, ALU.mult, r=[pn_, rn, "gains"], w=[on_])
                        for i in range(2):
                            X.dma("sp", dst[2 * fc + i, 0:64, tsl(g)], o_[i * 64:(i + 1) * 64, :], r=[on_],
                                  w=["QT" if isq else "KT"], key="d_" + on_)
            elif gi < 8:
                for tb in range(NB):
                    p_, pn_ = pacc.get()
                    for kc in range(KC):
                        X.mm(p_[:], hT[:, kc, tb * 128:(tb + 1) * 128], w_[:, kc, :], kc == 0, kc == KC - 1,
                             r=[wn, "hT%d" % (tb // 4)], w=[pn_])
                    if gi < 6:
                        v_, vn_ = vst.get()
                        X.act(v_[:, :, 0:64], p_[:].rearrange("p (h d) -> p h d", d=64), AF.Copy, r=[pn_], w=[vn_])
                        h0 = (gi - 4) * 8
                        X.dma("sp", Sx["V"][tb * 128:(tb + 1) * 128, h0 * 65:(h0 + 8) * 65],
                              v_[:].rearrange("p h d -> p (h d)"), r=[vn_], w=["V"], key="d_" + vn_)
                    else:
                        o_, on_ = stg.get()
                        X.copy("dve", o_[:], p_[:], r=[pn_], w=[on_])
                        c0 = (gi - 6) * 512
                        X.dma("sp", Sx["VH"][tb * 128:(tb + 1) * 128, c0:c0 + 512], o_[:], r=[on_], w=["VH"],
                              key="d_" + on_)
                if gi == 5:
                    pf_, pfn_ = pacc.get()
                    for tb in range(NB):
                        for kc in range(KC):
                            X.mm(pf_[:, tb * 16:(tb + 1) * 16], hT[:, kc, tb * 128:(tb + 1) * 128], wff[:, kc, :],
                                 kc == 0, kc == KC - 1, r=["wff", "hT%d" % (tb // 4)], w=[pfn_])
                    t_, tn_ = f32t.get()
                    X.tt("dve", t_[:].rearrange("p (b h) -> p b h", h=16), pf_[:].rearrange("p (b h) -> p b h", h=16),
                         T["fbias"][:, l:l + 1, :].broadcast_to([128, NB, 16]), ALU.add, r=[pfn_, "fbias"], w=[tn_])
                    X.act(t_[:], t_[:], AF.Sigmoid, r=[tn_], w=[tn_])
                    X.act(T["LF"][:].rearrange("p b h -> p (b h)"), t_[:], AF.Ln, r=[tn_], w=["LF"])
            elif gi < 12:
                for g in range(NG):
                    for jj in range(2):
                        j = (gi - 8) * 2 + jj
                        pz, pzn = mm_fm(w_, wn, 2 * jj, g)
                        pq, pqn = mm_fm(w_, wn, 2 * jj + 1, g)
                        sig, sign = f32t.get()
                        X.act(sig[:], pz[:], AF.Sigmoid, r=[pzn], w=[sign])
                        sq_, sqn_ = f32t.get()
                        X.act(sq_[:], pq[:], AF.Sigmoid, r=[pqn], w=[sqn_])
                        X.tt("dve", sq_[:], pq[:], sq_[:], ALU.mult, r=[pqn, sqn_], w=[sqn_])
                        f_, fn_ = f32t.get()
                        X.ts("dve", f_[:], sig[:], T["OML"][:, j, l:l + 1], T["LB"][:, j, l:l + 1], ALU.mult, ALU.add,
                             r=[sign, "LBt"], w=[fn_])
                        X.s.add("dve", (lambda f_: lambda e: e.tensor_scalar_max(f_[:], f_[:], 1e-20))(f_), r=[fn_], w=[fn_])
                        X.act(f_[:], f_[:], AF.Ln, r=[fn_], w=[fn_])
                        kk, kkn = f32t.get()
                        X.ts("dve", kk[:], sig[:], T["NOML"][:, j, l:l + 1], T["OML"][:, j, l:l + 1], ALU.mult, ALU.add,
                             r=[sign, "LBt"], w=[kkn])
                        G_, Gn = f32t.get()
                        X.s.add("dve", (lambda G_, f_: lambda e: e.tensor_tensor_scan(
                            out=G_[:], data0=T["resetm"][:], data1=f_[:], initial=0.0, op0=ALU.mult, op1=ALU.add))(G_, f_),
                            r=[fn_, "resetm"], w=[Gn])
                        G3 = G_[:].rearrange("p (c i) -> p c i", i=CH)
                        d1, d1n = f32t.get()
                        X.tt("dve", d1[:].rearrange("p (c i) -> p c i", i=CH), G3, G3[:, :, CH // 2 - 1:CH // 2].broadcast_to([128, NCS, CH]),
                             ALU.subtract, r=[Gn], w=[d1n])
                        d3, d3n = f32t.get()
                        X.tt("dve", d3[:].rearrange("p (c i) -> p c i", i=CH), G3, G3[:, :, CH - 1:CH].broadcast_to([128, NCS, CH]),
                             ALU.subtract, r=[Gn], w=[d3n])
                        X.act(T["EGL"][:, j, g * NCS:(g + 1) * NCS], G3[:, :, CH - 1], AF.Exp, r=[Gn], w=["EGL"])
                        e1, e1n = f32t.get()
                        X.act(e1[:], d1[:], AF.Exp, r=[d1n], w=[e1n])
                        X.act(d1[:], d1[:], AF.Exp, r=[d1n], w=[d1n], scale=-1.0)
                        X.act(d3[:], d3[:], AF.Exp, r=[d3n], w=[d3n], scale=-1.0)
                        X.act(G_[:], G_[:], AF.Exp, r=[Gn], w=[Gn])
                        rows = slice(j * 128, (j + 1) * 128)
                        for (a_, an_, b_, bn_, dstn) in [(sq_, sqn_, e1, e1n, "QM"), (sq_, sqn_, G_, Gn, "QI"),
                                                         (kk, kkn, d1, d1n, "KM"), (kk, kkn, d3, d3n, "KL")]:
                            o_, on_ = stg.get()
                            X.tt("dve", o_[:], a_[:], b_[:], ALU.mult, r=[an_, bn_], w=[on_])
                            X.dma("sp", Sx[dstn][rows, tsl(g)], o_[:], r=[on_], w=[dstn], key="d_" + on_)
            elif gi < 16:
                for g in range(NG):
                    for jj in range(2):
                        j = (gi - 12) * 2 + jj
                        pg_, pgn = mm_fm(w_, wn, 2 * jj, g)
                        pb_, pbn = mm_fm(w_, wn, 2 * jj + 1, g)
                        a_, an_ = f32t.get()
                        X.act(a_[:], pg_[:], AF.Sigmoid, r=[pgn], w=[an_])
                        b_, bn_ = f32t.get()
                        X.act(b_[:], pb_[:], AF.Sigmoid, r=[pbn], w=[bn_])
                        X.tt("dve", a_[:], pg_[:], a_[:], ALU.mult, r=[pgn, an_], w=[an_])
                        o_, on_ = stg.get()
                        X.tt("pool", o_[:], a_[:], b_[:], ALU.mult, r=[an_, bn_], w=[on_])
                        X.dma("sp", Sx["GG"][j * 128:(j + 1) * 128, tsl(g)], o_[:], r=[on_], w=["GG"], key="d_" + on_)
            else:
                for g in range(NG):
                    for ci in range(4):
                        fc = (gi - 16) * 4 + ci
                        p_, pn_ = mm_fm(w_, wn, ci, g)
                        o_, on_ = stg.get()
                        X.act(o_[:], p_[:], AF.Sigmoid, r=[pn_], w=[on_])
                        X.dma("sp", Sx["SGA"][fc * 128:(fc + 1) * 128, tsl(g)], o_[:], r=[on_], w=["SGA"], key="d_" + on_)

        lft = xg[0][0:16].rearrange("p k t -> p (k t)")
        ft = xg[1][0:16].rearrange("p k t -> p (k t)")
        rrow = sb("rrow", [16, S], BF16)
        for q4 in range(8):
            p_, pn_ = pacc.get()
            for i in range(4):
                tb = q4 * 4 + i
                X.s.add("pe", (lambda p_, i, tb: lambda e: e.transpose(
                    p_[0:16, i * 128:(i + 1) * 128], T["LF"][:, tb, :], T["ident_f"][:]))(p_, i, tb),
                    r=["LF", "ident_f"], w=[pn_])
            X.copy("dve", lft[:, q4 * 512:(q4 + 1) * 512], p_[0:16, :], r=[pn_], w=["xg0"])
        for q4 in range(8):
            ini = 0.0 if q4 == 0 else ft[:, q4 * 512 - 1:q4 * 512]
            X.s.add("dve", (lambda q4, ini: lambda e: e.tensor_tensor_scan(
                out=ft[:, q4 * 512:(q4 + 1) * 512], data0=T["ones_f"][0:16, :], data1=lft[:, q4 * 512:(q4 + 1) * 512],
                initial=ini, op0=ALU.mult, op1=ALU.add))(q4, ini), r=["xg0", "xg1"], w=["xg1"])
        X.memset("pool", rrow[:, 0:128], 0.0, w=["rrow"])
        X.copy("dve", rrow[:, 128:].rearrange("h (b i) -> h b i", i=128),
               ft[:, 0:31 * 128].rearrange("h (b i) -> h b i", i=128)[:, :, 127:128].broadcast_to([16, 31, 128]),
               r=["xg1", "rrow"], w=["rrow"])
        X.dma("sp", Sx["QT"][:, 64, :], rrow[:], r=["rrow"], w=["QT"], key="d_rrow")
        p_, pn_ = pacc.get()
        for tb in range(NB):
            X.s.add("pe", (lambda p_, tb: lambda e: e.transpose(
                p_[:, tb * 16:(tb + 1) * 16], ft[:, tb * 128:(tb + 1) * 128], T["ident_f"][0:16, 0:16]))(p_, tb),
                r=["xg1", "ident_f"], w=[pn_])
        X.s.add("dve", (lambda p_: lambda e: e.tensor_scalar(
            out=T["NF"][:].rearrange("p b h -> p (b h)"), in0=p_[:], scalar1=-1.0, scalar2=None, op0=ALU.mult))(p_),
            r=[pn_], w=["NF"])
        if "NFd" in self.dbg:
            nfd = self.dscr("NFd", [128, NB * 16], F32)
            X.dma("sp", nfd, T["NF"][:].rearrange("p b h -> p (b h)"), r=["NF"], w=["NFd"])
            egd = self.dscr("EGLd", [128, KC * (S // CH)], F32)
            X.dma("sp", egd, T["EGL"][:].rearrange("p j c -> p (j c)"), r=["EGL"], w=["EGLd"])


    def phase_H(self, s, sb, ps):
        I, T, Sx = self.I, self.T, self.Sx
        l = self.l
        X = Ops(s)
        ones_b = sb("ones_b", [128, 128], BF16)
        X.memset("pool", ones_b[:], 1.0, w=["ones_b"])
        segb = {nm: Rot(sb, "sg" + nm, 2, [128, KC, 512], BF16) for nm in ["QM", "KM", "QI", "KL", "GG"]}
        HV = NCS // 2
        vsg = Rot(sb, "vsg", 2, [CH, HV, D], BF16)
        St = sb("St", [128, KC, 128], F32)
        Sb = Rot(sb, "Sb", 2, [128, KC, 128], BF16)
        X.memset("pool", St[:], 0.0, w=["St%d" % j for j in range(KC)])
        sb0, sb0n = Sb.get()
        X.memset("pool", sb0[:], 0.0, w=[sb0n])
        pA = Rot(ps, "pA", 2, [CH, KC * CH], F32)
        pK = Rot(ps, "pK", 2, [CH, 1024], BF16)
        pU = Rot(ps, "pU", 2, [128, 512], F32)
        pO = Rot(ps, "pO", 1, [128, 512], F32)
        pN = Rot(ps, "pN", 1, [128, 512], F32)
        Am = Rot(sb, "Am", 2, [CH, KC, CH], BF16)
        kh = Rot(sb, "kh", 2, [CH, KC, 128], BF16)
        obuf = Rot(sb, "obuf", 2, [128, KC, 512], F32)
        osq = Rot(sb, "osq", 2, [128, 512], BF16)
        rt = Rot(sb, "hrt", 2, [128, 512], F32)
        t1 = Rot(sb, "ht1", 2, [128, 512], F32)
        ostg = Rot(sb, "ostg", 3, [128, 512], BF16)
        fmv = lambda nm: Sx[nm].rearrange("(j p) t -> p j t", p=128)
        cur = None

        def load_seg(sg):
            d = {}
            for nm in ["QM", "KM", "QI", "KL", "GG"]:
                t_, n_ = segb[nm].get()
                X.dma("sp", t_[:], fmv(nm)[:, :, sg * 512:(sg + 1) * 512], r=[nm], w=[n_])
                d[nm] = (t_, n_)
            return d

        def load_v(hv):
            v_, vn_ = vsg.get()
            X.dma("sp", v_[:], Sx["VH"][hv * 256:(hv + 1) * 256, :].rearrange("(c s) d -> s c d", s=CH), r=["VH"], w=[vn_])
            return (v_, vn_)

        def front(d, c):
            cs = slice(c * CH, (c + 1) * CH)
            pa, pan = pA.get()
            for j in range(KC):
                X.mm(pa[:, j * CH:(j + 1) * CH], d["KM"][0][:, j, cs], d["QM"][0][:, j, cs], True, True,
                     r=[d["KM"][1], d["QM"][1]], w=[pan])
            am, amn = Am.get()
            X.tt("dve", am[:], pa[:].rearrange("s (j t) -> s j t", t=CH),
                 T["tri64"][0:CH, 0:CH].unsqueeze(1).broadcast_to([CH, KC, CH]), ALU.mult, r=[pan], w=[amn])
            pk, pkn = pK.get()
            for j in range(KC):
                X.s.add("pe", (lambda pk, j, cs: lambda e: e.transpose(
                    pk[:, j * 128:(j + 1) * 128], d["KL"][0][:, j, cs], T["ident_b"][:]))(pk, j, cs),
                    r=[d["KL"][1]], w=[pkn])
            k_, kn_ = kh.get()
            X.act(k_[:].rearrange("s j k -> s (j k)"), pk[:], AF.Copy, r=[pkn], w=[kn_])
            return (am, amn, k_, kn_)

        sbc = (sb0, sb0n)
        nxt = load_seg(0)
        vnext = load_v(0)
        fr = front(nxt, 0)
        for sg in range(NG):
            d = nxt
            if sg + 1 < NG:
                nxt = load_seg(sg + 1)
            ob, obn = obuf.get()
            for c in range(NCS):
                cs = slice(c * CH, (c + 1) * CH)
                am, amn, k_, kn_ = fr
                if c % HV == 0:
                    v_, vn_ = vnext
                    hv = sg * 2 + c // HV
                    if hv + 1 < 2 * NG:
                        vnext = load_v(hv + 1)
                cv = c % HV
                pus = [pU.get(), pU.get()]
                for j in range(KC):
                    pu, pun = pus[j // 4]
                    X.mm(pu[:, (j % 4) * 128:(j % 4 + 1) * 128], k_[:, j, :], v_[:, cv, j * 128:(j + 1) * 128], True, True,
                         r=[kn_, vn_], w=[pun])
                if c + 1 < NCS:
                    fr = front(d, c + 1)
                elif sg + 1 < NG:
                    fr = front(nxt, 0)
                po, pon = pO.get()
                for j in range(KC):
                    X.mm(po[:, j * CH:(j + 1) * CH], v_[:, cv, j * 128:(j + 1) * 128], am[:, j, :], True, False,
                         r=[vn_, amn], w=[pon])
                    X.mm(po[:, j * CH:(j + 1) * CH], sbc[0][:, j, :], d["QI"][0][:, j, cs], False, True,
                         r=[sbc[1], d["QI"][1]], w=[pon])
                X.act(ob[:, :, cs], po[:, 0:KC * CH].rearrange("p (j t) -> p j t", t=CH), AF.Copy, r=[pon], w=[obn])
                cidx = sg * NCS + c
                for j in range(KC):
                    pu, pun = pus[j // 4]
                    X.stt("dve", St[:, j, :], St[:, j, :], T["EGL"][:, j, cidx:cidx + 1],
                          pu[:, (j % 4) * 128:(j % 4 + 1) * 128], ALU.mult, ALU.add, r=["St%d" % j, pun], w=["St%d" % j])
                sbc = Sb.get()
                X.copy("pool", sbc[0][:], St[:], r=["St%d" % j for j in range(KC)], w=[sbc[1]])
            for j in range(KC):
                q_, qn_ = osq.get()
                X.tt("pool", q_[:], ob[:, j, :], ob[:, j, :], ALU.mult, r=[obn], w=[qn_])
                n_, nn_ = pN.get()
                X.mm(n_[:], ones_b[:], q_[:], True, True, r=[qn_, "ones_b"], w=[nn_])
                r_, rn_ = rt.get()
                X.act(r_[:], n_[:], AF.Ln, r=[nn_], w=[rn_], bias=self.cbias(float(128 * EPS)))
                X.act(r_[:], r_[:], AF.Exp, r=[rn_], w=[rn_], scale=-0.5)
                a_, an_ = t1.get()
                X.stt("dve", a_[:], ob[:, j, :], T["ogainS"][:, l:l + 1], r_[:], ALU.mult, ALU.mult, r=[obn, rn_], w=[an_])
                o_, on_ = ostg.get()
                X.tt("dve", o_[:], a_[:], d["GG"][0][:, j, :], ALU.mult, r=[an_, d["GG"][1]], w=[on_])
                X.dma("sp", Sx["OBG"][j * 128:(j + 1) * 128, sg * 512:(sg + 1) * 512], o_[:], r=[on_], w=["OBG"],
                      key="d_" + on_)

    def phase_T(self, s, sb, ps):
        I, T, Sx = self.I, self.T, self.Sx
        l = self.l
        X = Ops(s)
        Vsb = sb("Vsb", [128, NB, 16 * 65], BF16)
        for q in range(4):
            X.dma("sp", Vsb[:, q * 8:(q + 1) * 8, :],
                  Sx["V"][q * 1024:(q + 1) * 1024, :].rearrange("(b p) c -> p b c", p=128), r=["V"], w=["Vsb%d" % q],
                  key="d_Vsb%d" % q)
        WO = sb("WO", [128, KC, D], BF16)
        X.dma("pool", WO[:], I["w_out"][l], r=[], w=["WO"])
        ktb = Rot(sb, "ktb", 2, [65, S], BF16)
        qtb = Rot(sb, "qtb", 2, [65, 512], BF16)
        ptb = Rot(sb, "ptb", 4, [128, 512], BF16)
        oab = Rot(sb, "oab", 2, [128, 4, D], BF16)
        rec = Rot(sb, "rec", 2, [128, 4], F32)
        pS = Rot(ps, "pS", 3, [128, 512], F32)
        pOo = Rot(ps, "pOo", 2, [128, 4, 65], F32)
        pTr = Rot(ps, "pTr", 1, [128, 512], BF16)
        pY = Rot(ps, "pY", 2, [128, 512], F32)
        sga = Rot(sb, "sga", 1, [128, KC, 512], BF16)
        obg = Rot(sb, "obg", 1, [128, KC, 512], BF16)
        mt = Rot(sb, "mt", 2, [128, 512], F32)
        yT = Rot(sb, "yT", 1, [128, KC, 512], BF16)
        xg = Rot(sb, "xgT", 1, [128, KC, 512], F32)
        fmv = lambda ap: ap.rearrange("(j p) t -> p j t", p=128)
        g1c = 16
        for G in range(NG):
            nk = 4 * G + 4
            oa, oan = oab.get()
            LA = 2
            tiles = [(h, j) for h in range(16) for j in range(nk)]
            hd = {}

            def load_head(h):
                kt, ktn = ktb.get()
                X.dma("sp", kt[:, 0:nk * 128], Sx["KT"][h, :, 0:nk * 128], r=["KT", "KT64"], w=[ktn])
                qt, qtn = qtb.get()
                X.dma("sp", qt[:], Sx["QT"][h, :, G * 512:(G + 1) * 512], r=["QT"], w=[qtn])
                hd[h] = [kt, ktn, qt, qtn, None]

            def qk(h, j):
                if j == 0:
                    if h + 1 < 16:
                        load_head(h + 1)
                kt, ktn, qt, qtn, _ = hd[h]
                nq0 = max(0, j - 4 * G)
                N = 512 - 128 * nq0
                p_, pn_ = pS.get()
                diag = j >= 4 * G
                if diag:
                    X.mm(p_[:, 0:N], T["ident_b"][:], T["maskneg"][:, 0:N], True, False, r=[], w=[pn_])
                X.mm(p_[:, 0:N], kt[:, j * 128:(j + 1) * 128], qt[:, nq0 * 128:512], not diag, True,
                     r=[ktn, qtn], w=[pn_])
                return (p_, pn_, nq0, N)

            load_head(0)
            pend = [qk(*tiles[i]) for i in range(min(LA, len(tiles)))]
            for i, (h, j) in enumerate(tiles):
                if i + LA < len(tiles):
                    pend.append(qk(*tiles[i + LA]))
                p_, pn_, nq0, N = pend.pop(0)
                if j == 0:
                    hd[h][4] = pOo.get()
                po, pon = hd[h][4]
                pt, ptn = ptb.get()
                X.act(pt[:, 0:N], p_[:, 0:N], AF.Exp, r=[pn_, "NF"], w=[ptn], bias=T["NF"][:, j, h:h + 1])
                for qb in range(nq0, 4):
                    X.mm(po[:, qb, :], pt[:, (qb - nq0) * 128:(qb - nq0 + 1) * 128], Vsb[:, j, h * 65:(h + 1) * 65],
                         j == 0 and qb == 0, j == 4 * G + qb, r=[ptn, "Vsb%d" % (j // 8)], w=[pon], skip=True)
                if j == nk - 1:
                    rc, rcn = rec.get()
                    X.s.add("dve", (lambda rc, po: lambda e: e.reciprocal(rc[:], po[:, :, 64]))(rc, po), r=[pon], w=[rcn])
                    X.tt("dve", oa[:, :, h * 64:(h + 1) * 64], po[:, :, 0:64], rc[:].unsqueeze(2).broadcast_to([128, 4, 64]),
                         ALU.mult, r=[pon, rcn], w=[oan])
                    del hd[h]
            if "OAd" in self.dbg:
                if G == 0:
                    self.oad = self.dscr("OAd", [S, D], BF16)
                X.dma("sp", self.oad[G * 512:(G + 1) * 512, :].rearrange("(q p) d -> p q d", p=128), oa[:], r=[oan],
                      w=["OAd"], key="d_oad")
            tsl = slice(G * 512, (G + 1) * 512)
            sg_, sgn = sga.get()
            X.dma("sp", sg_[:], fmv(Sx["SGA"])[:, :, tsl], r=["SGA"], w=[sgn])
            ob_, obn = obg.get()
            X.dma("sp", ob_[:], fmv(Sx["OBG"])[:, :, tsl], r=["OBG"], w=[obn])
            x_, xn_ = xg.get()
            X.dma("sp", x_[:], fmv(Sx["XT"])[:, :, tsl], r=["XT"], w=[xn_])
            y_, yn_ = yT.get()
            for fc in range(KC):
                tr, trn = pTr.get()
                for qb in range(4):
                    X.s.add("pe", (lambda tr, qb, fc, oa: lambda e: e.transpose(
                        tr[:, qb * 128:(qb + 1) * 128], oa[:, qb, fc * 128:(fc + 1) * 128], T["ident_b"][:]))(tr, qb, fc, oa),
                        r=[oan], w=[trn])
                m_, mn_ = mt.get()
                X.tt("dve", m_[:], tr[:], sg_[:, fc, :], ALU.mult, r=[trn, sgn], w=[mn_])
                X.tt("pool", y_[:, fc, :], m_[:], ob_[:, fc, :], ALU.add, r=[mn_, obn], w=[yn_])
            for dc in range(KC):
                py, pyn = pY.get()
                for fc in range(KC):
                    X.mm(py[:], WO[:, fc, dc * 128:(dc + 1) * 128], y_[:, fc, :], fc == 0, fc == KC - 1,
                         r=["WO", yn_], w=[pyn])
                X.stt("dve", x_[:, dc, :], py[:], T["ADA"][:, l, g1c + dc:g1c + dc + 1], x_[:, dc, :], ALU.mult, ALU.add,
                      r=[pyn, xn_], w=[xn_])
            X.dma("sp", fmv(Sx["XT"])[:, :, tsl], x_[:], r=[xn_], w=["XT"], key="d_st" + xn_)

    def phase_M(self, s, sb, ps):
        I, T, Sx = self.I, self.T, self.Sx
        l = self.l
        X = Ops(s)
        moe = (l % 2 == 1)
        li = l // 2
        NFC = 28 if moe else 22
        ne = NE if moe else 1
        last = (l == self.n_layers - 1)
        fmv = lambda ap: ap.rearrange("(j p) t -> p j t", p=128)
        hT = sb("hT2", [128, KC, 1024], BF16)
        xa = sb("xa", [128, KC, 1024], F32)
        hid = sb("hid", [128, NFC, 1024], BF16)
        sqr = Rot(sb, "msq", 2, [128, 512], F32)
        nt = Rot(sb, "mnt", 2, [128, 512], F32)
        rstd = Rot(sb, "mrs", 1, [128, 512], F32)
        pss = Rot(ps, "mpss", 1, [128, 512], F32)
        pG = Rot(ps, "pG", 2, [128, 512], F32)
        pUu = Rot(ps, "pUu", 2, [128, 512], F32)
        pD = Rot(ps, "pD", 2, [128, 512], F32)
        wg = Rot(sb, "wg", 2, [128, KC, 128], BF16)
        wu = Rot(sb, "wu", 2, [128, KC, 128], BF16)
        wd = Rot(sb, "wd", 2, [128, NFC, 128], BF16)
        wgs = Rot(sb, "wgs", 2, [128, KC, 128], F32)
        wus = Rot(sb, "wus", 2, [128, KC, 128], F32)
        NH = NFC // 2
        wds = Rot(sb, "wds", 2, [128, NH, 128], F32)
        sgt = Rot(sb, "sgt", 2, [128, 512], F32)
        tmpd = Rot(sb, "tmpd", 2, [128, 512], F32)
        if moe:
            wr = sb("wr", [128, KC, NE], F32)
            X.dma("sp", wr[:], I["w_router"][li], r=[], w=["wr"])
            h32 = Rot(sb, "h32", 1, [128, 512], F32)
            pL = Rot(ps, "pL", 1, [128, 512], F32)
            CT = sb("CT", [8, 1024], F32)
            CBr = Rot(sb, "CBe", 2, [128, 1024], BF16)
            lg = sb("lg", [128, 8, NE], F32)
            small = {nm: sb("sm_" + nm, [128, 8, NE], F32) for nm in ["eq", "l2", "sel", "ex", "w"]}
            m1 = sb("m1", [128, 8], F32)
            m2 = sb("m2", [128, 8], F32)
            ssum = sb("ssum", [128, 8], F32)
        A2, Bc, g2c = T["A2"], 24, 40
        for SG in range(4):
            tsl = slice(SG * 1024, (SG + 1) * 1024)
            X.dma("sp", xa[:], fmv(Sx["XT"])[:, :, tsl], r=["XT"], w=["xa"])
            if moe:
                pl, pln = pL.get()
            for hf in range(2):
                hs = slice(hf * 512, (hf + 1) * 512)
                p_, pn_ = pss.get()
                for kc in range(KC):
                    q_, qn_ = sqr.get()
                    X.tt("pool", q_[:], xa[:, kc, hs], xa[:, kc, hs], ALU.mult, r=["xa"], w=[qn_])
                    X.mm(p_[:], T["ones_f"][:, 0:128], q_[:], kc == 0, kc == KC - 1, r=[qn_], w=[pn_])
                r_, rn_ = rstd.get()
                X.act(r_[:], p_[:], AF.Ln, r=[pn_], w=[rn_], bias=self.cbias(float(D * EPS)))
                X.act(r_[:], r_[:], AF.Exp, r=[rn_], w=[rn_], scale=-0.5)
                for kc in range(KC):
                    t_, tn_ = nt.get()
                    X.tt("dve", t_[:], xa[:, kc, hs], r_[:], ALU.mult, r=["xa", rn_], w=[tn_])
                    X.act(hT[:, kc, hs], t_[:], AF.Identity, r=[tn_], w=["hT2_%d" % hf],
                          bias=T["ADA"][:, l, Bc + kc:Bc + kc + 1], scale=A2[:, l, kc:kc + 1])
                    if moe:
                        h_, hn_ = h32.get()
                        X.ts("dve", h_[:], t_[:], A2[:, l, kc:kc + 1], T["ADA"][:, l, Bc + kc:Bc + kc + 1], ALU.mult, ALU.add,
                             r=[tn_], w=[hn_])
                        for tb in range(4):
                            col = (hf * 4 + tb) * NE
                            X.mm(pl[:, col:col + NE], h_[:, tb * 128:(tb + 1) * 128], wr[:, kc, :],
                                 hf == 0 and kc == 0 and tb == 0, kc == KC - 1, r=[hn_, "wr"], w=[pln], skip=True)
            if moe:
                v3 = lambda t: t[:]
                bc = lambda t: t[:].unsqueeze(2).broadcast_to([128, 8, NE])
                X.tt("dve", lg[:], pl[:, 0:64].rearrange("p (b e) -> p b e", e=NE),
                     T["brout"][:, li:li + 1, :].broadcast_to([128, 8, NE]), ALU.add, r=[pln], w=["lg"])
                X.s.add("dve", lambda e: e.tensor_reduce(out=m1[:], in_=lg[:], axis=AX.X, op=ALU.max), r=["lg"], w=["m1"])
                X.tt("dve", small["eq"][:], lg[:], bc(m1), ALU.is_equal, r=["lg", "m1"], w=["eq"])
                X.stt("dve", small["l2"][:], small["eq"][:], -1e30, lg[:], ALU.mult, ALU.add, r=["eq", "lg"], w=["l2"])
                X.s.add("dve", lambda e: e.tensor_reduce(out=m2[:], in_=small["l2"][:], axis=AX.X, op=ALU.max), r=["l2"], w=["m2"])
                X.tt("dve", small["sel"][:], lg[:], bc(m2), ALU.is_ge, r=["lg", "m2"], w=["sel"])
                X.tt("dve", small["ex"][:], lg[:], bc(m1), ALU.subtract, r=["lg", "m1"], w=["ex"])
                X.act(small["ex"][:], small["ex"][:], AF.Exp, r=["ex"], w=["ex"])
                X.tt("dve", small["w"][:], small["ex"][:], small["sel"][:], ALU.mult, r=["ex", "sel"], w=["w"])
                X.s.add("dve", lambda e: e.tensor_reduce(out=ssum[:], in_=small["w"][:], axis=AX.X, op=ALU.add), r=["w"], w=["ssum"])
                X.s.add("dve", lambda e: e.reciprocal(ssum[:], ssum[:]), r=["ssum"], w=["ssum"])
                X.tt("dve", small["w"][:], small["w"][:], bc(ssum), ALU.mult, r=["w", "ssum"], w=["w"])
                for hf in range(2):
                    p_, pn_ = pD.get()
                    for tb in range(4):
                        X.s.add("pe", (lambda p_, tb, hf: lambda e: e.transpose(
                            p_[0:8, tb * 128:(tb + 1) * 128], small["w"][:, hf * 4 + tb, :], T["ident_f"][:]))(p_, tb, hf),
                            r=["w"], w=[pn_])
                    X.copy("dve", CT[:, hf * 512:(hf + 1) * 512], p_[0:8, :], r=[pn_], w=["CT"])
            for e_ in range(ne):
                if moe:
                    CB, cbn = CBr.get()
                    for hf in range(2):
                        p_, pn_ = pD.get()
                        X.mm(p_[:], T["sel8"][:, e_, :], CT[:, hf * 512:(hf + 1) * 512], True, True, r=["CT"], w=[pn_])
                        X.copy("dve", CB[:, hf * 512:(hf + 1) * 512], p_[:], r=[pn_], w=[cbn])
                for fcn in range(NFC):
                    g32, g32n = wgs.get()
                    u32, u32n = wus.get()
                    X.dma("sp", g32[:], (I["mwg"][li, e_, fcn] if moe else I["dwg"][li, fcn]), r=[], w=[g32n])
                    X.dma("sp", u32[:], (I["mwu"][li, e_, fcn] if moe else I["dwu"][li, fcn]), r=[], w=[u32n])
                    g_, gn_ = wg.get()
                    u_, un_ = wu.get()
                    X.act(g_[:], g32[:], AF.Copy, r=[g32n], w=[gn_])
                    X.copy("dve", u_[:], u32[:], r=[u32n], w=[un_])
                    for hf in range(2):
                        hs = slice(hf * 512, (hf + 1) * 512)
                        pg, pgn = pG.get()
                        for kc in range(KC):
                            X.mm(pg[:], g_[:, kc, :], hT[:, kc, hs], kc == 0, kc == KC - 1, r=[gn_, "hT2_%d" % hf], w=[pgn])
                        pu, pun = pUu.get()
                        for kc in range(KC):
                            X.mm(pu[:], u_[:, kc, :], hT[:, kc, hs], kc == 0, kc == KC - 1, r=[un_, "hT2_%d" % hf], w=[pun])
                        sg_, sgn = sgt.get()
                        X.act(sg_[:], pg[:], AF.Silu, r=[pgn], w=[sgn])
                        X.tt("dve", hid[:, fcn, hs], pu[:], sg_[:], ALU.mult, r=[pun, sgn], w=["hid"])
                for dc in range(KC):
                    d_, dn_ = wd.get()
                    src = (I["mwd"][li, e_][:, :, dc * 128:(dc + 1) * 128] if moe else I["dwd"][li][:, :, dc * 128:(dc + 1) * 128])
                    for hh in range(2):
                        d32, d32n = wds.get()
                        X.dma("sp", d32[:], src[:, hh * NH:(hh + 1) * NH, :], r=[], w=[d32n])
                        if hh == 0:
                            X.copy("dve", d_[:, hh * NH:(hh + 1) * NH, :], d32[:], r=[d32n], w=[dn_])
                        else:
                            X.act(d_[:, hh * NH:(hh + 1) * NH, :], d32[:], AF.Copy, r=[d32n], w=[dn_])
                    for hf in range(2):
                        hs = slice(hf * 512, (hf + 1) * 512)
                        pd, pdn = pD.get()
                        for fcn in range(NFC):
                            X.mm(pd[:], d_[:, fcn, :], hid[:, fcn, hs], fcn == 0, fcn == NFC - 1, r=[dn_, "hid"], w=[pdn])
                        gcol = T["ADA"][:, l, g2c + dc:g2c + dc + 1]
                        if moe:
                            t_, tn_ = tmpd.get()
                            X.stt("dve", t_[:], pd[:], gcol, CB[:, hs], ALU.mult, ALU.mult, r=[pdn, cbn], w=[tn_])
                            X.tt("pool", xa[:, dc, hs], xa[:, dc, hs], t_[:], ALU.add, r=[tn_, "xa"], w=["xa"])
                        else:
                            X.stt("dve", xa[:, dc, hs], pd[:], gcol, xa[:, dc, hs], ALU.mult, ALU.add, r=[pdn, "xa"], w=["xa"])
            dst = self.out if last else Sx["XT"]
            X.dma("sp", fmv(dst)[:, :, tsl], xa[:], r=["xa"], w=["XT"], key="d_stxa")


class Rot:
    def __init__(self, alloc, name, n, shape, dt):
        self.t = [(alloc("%s%d" % (name, i), shape, dt), "%s%d" % (name, i)) for i in range(n)]
        self.i = 0

    def get(self):
        t = self.t[self.i % len(self.t)]
        self.i += 1
        return t


class Ops:
    def __init__(self, s):
        self.s = s

    def mm(self, out, lhsT, rhs, start, stop, r, w, skip=False):
        if skip:
            self.s.add("pe", lambda e: e.matmul(out, lhsT, rhs, start=start, stop=stop, skip_group_check=True), r=r, w=w)
        else:
            self.s.add("pe", lambda e: e.matmul(out, lhsT, rhs, start=start, stop=stop), r=r, w=w)

    def act(self, out, in_, func, r, w, bias=None, scale=None):
        kw = {}
        if bias is not None:
            kw["bias"] = bias
        if scale is not None:
            kw["scale"] = scale
        self.s.add("act", lambda e: e.activation(out=out, in_=in_, func=func, **kw), r=r, w=w)

    def tt(self, eng, out, in0, in1, op, r, w):
        self.s.add(eng, lambda e: e.tensor_tensor(out=out, in0=in0, in1=in1, op=op), r=r, w=w)

    def ts(self, eng, out, in0, s1, s2, op0, op1, r, w):
        self.s.add(eng, lambda e: e.tensor_scalar(out=out, in0=in0, scalar1=s1, scalar2=s2, op0=op0, op1=op1), r=r, w=w)

    def stt(self, eng, out, in0, scalar, in1, op0, op1, r, w):
        self.s.add(eng, lambda e: e.scalar_tensor_tensor(out=out, in0=in0, scalar=scalar, in1=in1, op0=op0, op1=op1),
                   r=r, w=w)

    def copy(self, eng, out, in_, r, w):
        self.s.add(eng, lambda e: e.tensor_copy(out, in_), r=r, w=w)

    def memset(self, eng, ap, v, w):
        self.s.add(eng, lambda e: e.memset(ap, v), w=w)

    def dma(self, eng, out, in_, r, w, key=None):
        if key is None:
            key = "d_" + w[0]
        self.s.add(eng, lambda e: e.dma_start(out=out, in_=in_), r=r, w=w, sem=key)


def _consts():
    ident = np.eye(128, dtype=np.float32)
    bd64 = np.zeros((128, 128), np.float32)
    bd64[:64, :64] = 1.0
    bd64[64:, 64:] = 1.0
    ss, tt = np.meshgrid(np.arange(64), np.arange(64), indexing="ij")
    tri64 = (ss <= tt).astype(np.float32)
    maskneg = np.zeros((128, 512), np.float32)
    ss, tt = np.meshgrid(np.arange(128), np.arange(128), indexing="ij")
    maskneg[:, :128] = np.where(ss > tt, -30000.0, 0.0)
    resetm = np.ones((128, 512), np.float32)
    resetm[:, ::CH] = 0.0
    sel8 = np.zeros((8, NE, 128), np.float32)
    for e in range(NE):
        sel8[e, e, :] = 1.0
    return dict(ident=ident, bd64=bd64, tri64=tri64, maskneg=maskneg, resetm=resetm, sel8=sel8)


def _pf(a):
    a = np.asarray(a, np.float32)
    lead = a.shape[:-1]
    n = a.shape[-1] // 128
    a = a.reshape(lead + (n, 128))
    return np.ascontiguousarray(np.moveaxis(a, -1, 0))


def _wtiles(w, ncol):
    K, N = w.shape
    kc = K // 128
    return np.ascontiguousarray(w.reshape(kc, 128, N // ncol, ncol).transpose(2, 1, 0, 3))


def _layout_fns(inp):
    f = lambda k: np.asarray(inp[k], np.float32)
    o = {}
    o["w_ada"] = lambda: np.stack([_wtiles(f("w_ada")[l], 768) for l in range(DEPTH)])
    o["b_ada"] = lambda: np.ascontiguousarray(_pf(f("b_ada")))
    o["norm_mix"] = lambda: _pf(f("norm_mix"))
    o["norm_ffn"] = lambda: _pf(f("norm_ffn"))

    def w_in_groups():
        w_in = f("w_in")
        offs = np.cumsum([0, 1024, 1024, 1024, 16, 1024, 1024, 1024, 1024, 1024, 1024])
        fq, fk, fv, ff, hq, hf, hi, hg, ga, gb = [w_in[:, :, offs[i]:offs[i + 1]] for i in range(10)]
        groups = [fq[:, :, :512], fq[:, :, 512:], fk[:, :, :512], fk[:, :, 512:], fv[:, :, :512], fv[:, :, 512:],
                  hi[:, :, :512], hi[:, :, 512:]]
        sl = lambda a, j: a[:, :, j * 128:(j + 1) * 128]
        for j in range(0, 8, 2):
            groups.append(np.concatenate([sl(hf, j), sl(hq, j), sl(hf, j + 1), sl(hq, j + 1)], axis=2))
        for j in range(0, 8, 2):
            groups.append(np.concatenate([sl(hg, j), sl(gb, j), sl(hg, j + 1), sl(gb, j + 1)], axis=2))
        groups += [ga[:, :, :512], ga[:, :, 512:]]
        wr = np.concatenate(groups, axis=2)
        return np.stack([_wtiles(wr[l], 512) for l in range(DEPTH)])

    o["w_in"] = w_in_groups
    o["w_ff"] = lambda: np.ascontiguousarray(f("w_in")[:, :, 3072:3088].reshape(DEPTH, KC, 128, 16).transpose(0, 2, 1, 3))
    o["qgain"] = lambda: np.ascontiguousarray(np.tile(f("fox_q_gain"), (1, 2)).T)
    o["kgain"] = lambda: np.ascontiguousarray(np.tile(f("fox_k_gain"), (1, 2)).T)
    o["fbias"] = lambda: np.ascontiguousarray(np.broadcast_to(f("fox_f_bias")[None], (128, DEPTH, 16)))
    o["hg_lb"] = lambda: np.ascontiguousarray(_pf(f("hg_lb")).transpose(0, 2, 1))
    o["ogain"] = lambda: np.ascontiguousarray(f("hg_o_gain").T)
    o["w_out"] = lambda: np.ascontiguousarray(f("w_out").reshape(DEPTH, KC, 128, D).transpose(0, 2, 1, 3))
    o["w_router"] = lambda: np.ascontiguousarray(f("w_router").reshape(2, KC, 128, NE).transpose(0, 2, 1, 3))
    o["b_router"] = lambda: np.ascontiguousarray(np.broadcast_to(f("b_router")[None], (128, 2, NE)))
    o["dwg"] = lambda: np.stack([_wtiles(f("dense_w_gate")[i], 128) for i in range(2)])
    o["dwu"] = lambda: np.stack([_wtiles(f("dense_w_up")[i], 128) for i in range(2)])
    o["dwd"] = lambda: np.ascontiguousarray(f("dense_w_down").reshape(2, 22, 128, D).transpose(0, 2, 1, 3))
    o["mwg"] = lambda: np.stack([np.stack([_wtiles(f("moe_w_gate")[i, e], 128) for e in range(NE)]) for i in range(2)])
    o["mwu"] = lambda: np.stack([np.stack([_wtiles(f("moe_w_up")[i, e], 128) for e in range(NE)]) for i in range(2)])
    o["mwd"] = lambda: np.ascontiguousarray(f("moe_w_down").reshape(2, NE, 28, 128, D).transpose(0, 1, 3, 2, 4))
    for k, v in _consts().items():
        o[k] = (lambda v: lambda: v)(v)
    return o


def _layout_shared_subset(inp, keys):
    fns = _layout_fns(inp)
    return {k: fns[k]() for k in keys if k in fns}


def _layout_shared(inp):
    fns = _layout_fns(inp)
    return {k: fn() for k, fn in fns.items()}


def make_in_maps(inp, n_cores=8):
    shared = _layout_shared(inp)
    x = np.asarray(inp["x"], np.float32)
    c = np.asarray(inp["c"], np.float32)
    maps = []
    for b in range(n_cores):
        m = dict(shared)
        m["xT"] = np.ascontiguousarray(x[b].T)
        m["c"] = np.ascontiguousarray(c[b].reshape(KC, 128).T)
        maps.append(m)
    return maps


def kernel(**inp):
    mk = MK()
    nc = mk.build()
    maps = make_in_maps(inp)
    res = run_bass_kernel_spmd(nc, maps, core_ids=list(range(8)))
    out = np.stack([np.ascontiguousarray(r["yT"].T) for r in res.results], axis=0)
    return out.astype(np.float32)
```

```python
import numpy as np
from contextlib import ExitStack
import concourse.bass as bass
import concourse.mybir as mybir
from concourse.bass_utils import run_bass_kernel_spmd

F32 = mybir.dt.float32
BF16 = mybir.dt.bfloat16
AF = mybir.ActivationFunctionType
ALU = mybir.AluOpType
AX = mybir.AxisListType

S = 4096
D = 1024
KC = 8
NG = 8
NB = 32
DEPTH = 4
EPS = 1e-6
DFF = 2816
DFE = 3584
NE = 8
CH = 32
NCS = 512 // CH


class _Op:
    __slots__ = ("eng", "fn", "sem", "inc", "deps", "needed", "sigval", "idx")


class Sched:
    EPOCH = 12000

    def __init__(self, nc):
        self.nc = nc
        self.ops = []
        self.last_w = {}
        self.readers = {}
        self.neng = {}

    def add(self, eng, fn, r=(), w=(), sem=None):
        op = _Op()
        op.eng = eng
        op.fn = fn
        if sem is None:
            n = self.neng.get(eng, 0)
            self.neng[eng] = n + 1
            op.sem = "%s#%d" % (eng, n // self.EPOCH)
            op.inc = 1
            op.needed = False
        else:
            op.sem = sem
            op.inc = 16
            op.needed = True
        deps = {}

        def dep(o):
            if o is None:
                return
            if o.eng == "pe" and eng == "pe":
                return
            k = o.sem
            if k not in deps or deps[k].idx < o.idx:
                deps[k] = o

        for x in r:
            dep(self.last_w.get(x))
        for x in w:
            dep(self.last_w.get(x))
            rd = self.readers.get(x)
            if rd:
                for o in rd.values():
                    dep(o)
        op.idx = len(self.ops)
        op.deps = deps
        for o in deps.values():
            o.needed = True
        for x in r:
            self.readers.setdefault(x, {})[op.sem] = op
        for x in w:
            self.last_w[x] = op
            self.readers[x] = {}
        self.ops.append(op)
        return op

    def emit(self):
        nc = self.nc
        last = {}
        for op in self.ops:
            if op.fn is not None:
                last[op.sem] = op
        for op in last.values():
            op.needed = True
        cnt = {}
        for op in self.ops:
            if op.fn is not None and op.needed:
                cnt[op.sem] = cnt.get(op.sem, 0) + op.inc
                op.sigval = cnt[op.sem]
        keys = sorted(cnt.keys())
        sems = {k: nc.alloc_semaphore(name="s_" + k.replace("#", "_") + getattr(self, "uid", "")) for k in keys}
        with nc.Block() as block:

            def run(engname):
                def f(e):
                    waited = {}
                    for op in self.ops:
                        if op.eng != engname:
                            continue
                        for k, d in op.deps.items():
                            if waited.get(k, 0) >= d.sigval:
                                continue
                            e.wait_ge(sems[k], d.sigval)
                            waited[k] = d.sigval
                        if op.fn is not None:
                            ins = op.fn(e)
                            if op.needed:
                                ins.then_inc(sems[op.sem], op.inc)
                    for k in keys:
                        if waited.get(k, 0) < cnt[k]:
                            e.wait_ge(sems[k], cnt[k])
                return f

            block.tensor(run("pe"))
            block.scalar(run("act"))
            block.vector(run("dve"))
            block.gpsimd(run("pool"))
            block.sync(run("sp"))
        nc.all_engine_barrier()
        nc.clear_and_free_semaphores(list(sems.values()))
        nc.all_engine_barrier()


class MK:
    def __init__(self, n_layers=DEPTH, dbg=(), stop_after=None):
        self.n_layers = n_layers
        self.dbg = set(dbg)
        self.stop_after = stop_after
        self.nc = bass.Bass("TRN2", target_bir_lowering=False)
        self.es = ExitStack()
        self.uid = 0

    def din(self, name, shape, dt=F32):
        return self.nc.dram_tensor(name, list(shape), dt, kind="ExternalInput").ap()

    def dscr(self, name, shape, dt):
        kind = "ExternalOutput" if name in self.dbg else "Internal"
        return self.nc.dram_tensor(name, list(shape), dt, kind=kind).ap()

    def persist(self, name, shape, dt):
        return self.es.enter_context(self.nc.sbuf_tensor(name, list(shape), dt))

    CB = [float(D * EPS), float(64 * EPS), float(128 * EPS)]

    def cbias(self, v):
        i = self.CB.index(v)
        return self.T["cb"][:, i:i + 1]

    def phase(self, body):
        nc = self.nc
        with ExitStack() as es:
            s = Sched(nc)
            self.uid += 1
            u = "_u%d" % self.uid
            s.uid = u

            def sb(name, shape, dt=F32):
                return es.enter_context(nc.sbuf_tensor(name + u, list(shape), dt))

            def ps(name, shape, dt=F32):
                return es.enter_context(nc.psum_tensor(name + u, list(shape), dt))

            body(s, sb, ps)
            s.emit()

    def build(self):
        nc = self.nc
        L = self.n_layers
        shapes = {
            "xT": [D, S], "c": [128, KC], "w_ada": [DEPTH, 8, 128, KC, 768], "b_ada": [128, DEPTH, 48],
            "norm_mix": [128, DEPTH, KC], "norm_ffn": [128, DEPTH, KC], "w_in": [DEPTH, 18, 128, KC, 512],
            "w_ff": [DEPTH, 128, KC, 16], "qgain": [128, DEPTH], "kgain": [128, DEPTH], "fbias": [128, DEPTH, 16],
            "hg_lb": [128, KC, DEPTH], "ogain": [128, DEPTH], "w_out": [DEPTH, 128, KC, D],
            "w_router": [2, 128, KC, NE], "b_router": [128, 2, NE],
            "dwg": [2, 22, 128, KC, 128], "dwu": [2, 22, 128, KC, 128], "dwd": [2, 128, 22, D],
            "mwg": [2, NE, 28, 128, KC, 128], "mwu": [2, NE, 28, 128, KC, 128], "mwd": [2, NE, 128, 28, D],
            "ident": [128, 128], "bd64": [128, 128], "tri64": [64, 64], "maskneg": [128, 512],
            "resetm": [128, 512], "sel8": [8, NE, 128],
        }
        mk = self

        class _Lazy(dict):
            def __missing__(self, k):
                v = mk.din(k, shapes[k])
                self[k] = v
                return v

        I = _Lazy()
        self.I = I
        self.out = self.nc.dram_tensor("yT", [D, S], F32, kind="ExternalOutput").ap()

        Sx = {}
        Sx["XT"] = self.dscr("XT", [D, S], F32)
        Sx["QT"] = self.dscr("QT", [16, 65, S], BF16)
        Sx["KT"] = self.dscr("KT", [16, 65, S], BF16)
        Sx["V"] = self.dscr("V", [S, 16 * 65], BF16)
        Sx["QM"] = self.dscr("QM", [D, S], BF16)
        Sx["KM"] = self.dscr("KM", [D, S], BF16)
        Sx["QI"] = self.dscr("QI", [D, S], BF16)
        Sx["KL"] = self.dscr("KL", [D, S], BF16)
        Sx["VH"] = self.dscr("VH", [S, D], BF16)
        Sx["GG"] = self.dscr("GG", [D, S], BF16)
        Sx["SGA"] = self.dscr("SGA", [D, S], BF16)
        Sx["OBG"] = self.dscr("OBG", [D, S], BF16)
        self.Sx = Sx

        T = {}
        T["ident_f"] = self.persist("ident_f", [128, 128], F32)
        T["ident_b"] = self.persist("ident_b", [128, 128], BF16)
        T["ones_f"] = self.persist("ones_f", [128, 512], F32)
        T["bd64"] = self.persist("bd64_b", [128, 128], BF16)
        T["tri64"] = self.persist("tri64_f", [64, 64], F32)
        T["maskneg"] = self.persist("maskneg_b", [128, 512], BF16)
        T["resetm"] = self.persist("resetm_f", [128, 512], F32)
        T["sel8"] = self.persist("sel8_f", [8, NE, 128], F32)
        T["ADA"] = self.persist("ADA", [128, DEPTH, 48], F32)
        T["A1"] = self.persist("A1", [128, DEPTH, KC], F32)
        T["A2"] = self.persist("A2", [128, DEPTH, KC], F32)
        T["LB"] = self.persist("LB", [128, KC, DEPTH], F32)
        T["OML"] = self.persist("OML", [128, KC, DEPTH], F32)
        T["NOML"] = self.persist("NOML", [128, KC, DEPTH], F32)
        T["qgain"] = self.persist("qgain_s", [128, DEPTH], F32)
        T["kgain"] = self.persist("kgain_s", [128, DEPTH], F32)
        T["ogain"] = self.persist("ogain_s", [128, DEPTH], F32)
        T["kgain8"] = self.persist("kgain8", [128, DEPTH], F32)
        T["ogainS"] = self.persist("ogainS", [128, DEPTH], F32)
        T["fbias"] = self.persist("fbias_s", [128, DEPTH, 16], F32)
        T["brout"] = self.persist("brout_s", [128, 2, NE], F32)
        T["EGL"] = self.persist("EGL", [128, KC, S // CH], F32)
        T["cb"] = self.persist("cbias", [128, 8], F32)
        T["LF"] = self.persist("LF", [128, NB, 16], F32)
        T["NF"] = self.persist("NF", [128, NB, 16], F32)
        self.T = T

        self.phase(self.prologue)
        if self.stop_after == "prologue":
            return self.finish()
        if isinstance(self.stop_after, tuple) and self.stop_after[0] == "onlyM":
            self.l = self.stop_after[1]
            self.n_layers = self.l + 1
            self.phase(self.phase_M)
            return self.finish()
        for l in range(L):
            self.l = l
            self.phase(self.phase_A)
            if self.stop_after in (("A", l), ("A1", l)):
                return self.finish()
            self.phase(self.phase_H)
            if self.stop_after == ("H", l):
                return self.finish()
            self.phase(self.phase_T)
            if self.stop_after == ("T", l):
                return self.finish()
            self.phase(self.phase_M)
            if self.stop_after == ("M", l):
                return self.finish()
        return self.finish()

    def finish(self):
        self.es.close()
        return self.nc

    def prologue(self, s, sb, ps):
        I, T, Sx = self.I, self.T, self.Sx
        stage = sb("pstage", [128, 512], F32)
        stage2 = sb("pstage2", [128, 128], F32)
        s.add("sp", lambda e: e.dma_start(out=T["ident_f"][:], in_=I["ident"]), w=["ident_f"], sem="d0_1")
        s.add("sp", lambda e: e.dma_start(out=stage2[:], in_=I["bd64"]), w=["stage2"], sem="d0_2")
        s.add("sp", lambda e: e.dma_start(out=T["tri64"][:], in_=I["tri64"]), w=["tri64"], sem="d0_3")
        s.add("sp", lambda e: e.dma_start(out=stage[:], in_=I["maskneg"]), w=["stage"], sem="d0_4")
        s.add("sp", lambda e: e.dma_start(out=T["resetm"][:], in_=I["resetm"]), w=["resetm"], sem="d0_5")
        s.add("sp", lambda e: e.dma_start(out=T["sel8"][:], in_=I["sel8"]), w=["sel8"], sem="d0_6")
        s.add("dve", lambda e: e.tensor_copy(T["ident_b"][:], T["ident_f"][:]), r=["ident_f"], w=["ident_b"])
        s.add("dve", lambda e: e.tensor_copy(T["bd64"][:], stage2[:]), r=["stage2"], w=["bd64"])
        s.add("dve", lambda e: e.tensor_copy(T["maskneg"][:], stage[:]), r=["stage"], w=["maskneg"])
        s.add("pool", lambda e: e.memset(T["ones_f"][:], 1.0), w=["ones_f"])
        for i, v in enumerate(self.CB):
            s.add("pool", (lambda i, v: lambda e: e.memset(T["cb"][:, i:i + 1], v))(i, v), w=["cb"])
        for nm in ["qgain", "kgain", "ogain", "fbias"]:
            s.add("sp", (lambda nm: lambda e: e.dma_start(out=T[nm][:], in_=I[nm]))(nm), w=[nm], sem="d0_" + nm)
        s.add("sp", lambda e: e.dma_start(out=T["brout"][:], in_=I["b_router"]), w=["brout"], sem="d0_8")
        s.add("dve", lambda e: e.tensor_scalar(out=T["kgain8"][:], in0=T["kgain"][:], scalar1=8.0, scalar2=None,
                                               op0=ALU.mult), r=["kgain"], w=["kgain8"])
        s.add("dve", lambda e: e.tensor_scalar(out=T["ogainS"][:], in0=T["ogain"][:], scalar1=float(128 ** 0.5),
                                               scalar2=None, op0=ALU.mult), r=["ogain"], w=["ogainS"])
        nmix = sb("nmix", [128, DEPTH, KC], F32)
        nffn = sb("nffn", [128, DEPTH, KC], F32)
        bada = sb("bada", [128, DEPTH, 48], F32)
        cin = sb("cin", [128, KC], F32)
        cact = sb("cact", [128, KC], F32)
        lbin = sb("lbin", [128, KC, DEPTH], F32)
        s.add("sp", lambda e: e.dma_start(out=nmix[:], in_=I["norm_mix"]), w=["nmix"], sem="d0_9")
        s.add("sp", lambda e: e.dma_start(out=nffn[:], in_=I["norm_ffn"]), w=["nffn"], sem="d0_10")
        s.add("sp", lambda e: e.dma_start(out=bada[:], in_=I["b_ada"]), w=["bada"], sem="d0_11")
        s.add("sp", lambda e: e.dma_start(out=cin[:], in_=I["c"]), w=["cin"], sem="d0_12")
        s.add("sp", lambda e: e.dma_start(out=lbin[:], in_=I["hg_lb"]), w=["lbin"], sem="d0_13")
        s.add("sp", lambda e: e.dma_start(out=Sx["XT"], in_=I["xT"]), w=["XT"], sem="d1")
        onesb = sb("onesb", [16, S], BF16)
        s.add("pool", lambda e: e.memset(onesb[:], 1.0), w=["onesb"])
        s.add("sp", lambda e: e.dma_start(out=Sx["KT"][:, 64, :], in_=onesb[:]), r=["onesb"], w=["KT64"], sem="d1b")
        s.add("act", lambda e: e.activation(out=cact[:], in_=cin[:], func=AF.Silu), r=["cin"], w=["cact"])
        wa = [sb("wa%d" % i, [128, KC, 768], F32) for i in range(2)]
        pada = ps("pada", [128, 512], F32)
        n = 0
        for l in range(DEPTH):
            for blk in range(8):
                b = n % 2
                n += 1
                s.add("sp", (lambda b, l, blk: lambda e: e.dma_start(out=wa[b][:], in_=I["w_ada"][l, blk]))(b, l, blk),
                      w=["wa%d" % b], sem="dwa%d" % b)
                for m in range(6):
                    col = l * 48 + blk * 6 + m
                    for kc in range(KC):
                        s.add("pe", (lambda b, m, kc, col: lambda e: e.matmul(
                            pada[:, col:col + 1], wa[b][:, kc, m * 128:(m + 1) * 128], cact[:, kc:kc + 1],
                            start=(kc == 0), stop=(kc == KC - 1)))(b, m, kc, col),
                            r=["wa%d" % b, "cact"], w=["pada"])
        s.add("dve", lambda e: e.tensor_tensor(
            out=T["ADA"][:].rearrange("p l m -> p (l m)"), in0=pada[:, 0:DEPTH * 48],
            in1=bada[:].rearrange("p l m -> p (l m)"), op=ALU.add), r=["pada", "bada"], w=["ADA"])
        tmpA = sb("tmpA", [128, DEPTH, KC], F32)
        s.add("dve", lambda e: e.tensor_scalar(out=tmpA[:], in0=T["ADA"][:, :, 8:16], scalar1=1.0, scalar2=32.0,
                                               op0=ALU.add, op1=ALU.mult), r=["ADA"], w=["tmpA"])
        s.add("dve", lambda e: e.tensor_tensor(out=T["A1"][:], in0=tmpA[:], in1=nmix[:], op=ALU.mult),
              r=["tmpA", "nmix"], w=["A1"])
        tmpB = sb("tmpB", [128, DEPTH, KC], F32)
        s.add("dve", lambda e: e.tensor_scalar(out=tmpB[:], in0=T["ADA"][:, :, 32:40], scalar1=1.0, scalar2=32.0,
                                               op0=ALU.add, op1=ALU.mult), r=["ADA"], w=["tmpB"])
        s.add("dve", lambda e: e.tensor_tensor(out=T["A2"][:], in0=tmpB[:], in1=nffn[:], op=ALU.mult),
              r=["tmpB", "nffn"], w=["A2"])
        lbe = sb("lbe", [128, KC, DEPTH], F32)
        lbs = sb("lbs", [128, KC], F32)
        lbr = sb("lbr", [128, KC], F32)
        lbsm = sb("lbsm", [128, KC, DEPTH], F32)
        s.add("act", lambda e: e.activation(out=lbe[:], in_=lbin[:], func=AF.Exp), r=["lbin"], w=["lbe"])
        s.add("dve", lambda e: e.tensor_reduce(out=lbs[:], in_=lbe[:], axis=AX.X, op=ALU.add), r=["lbe"], w=["lbs"])
        s.add("dve", lambda e: e.reciprocal(lbr[:], lbs[:]), r=["lbs"], w=["lbr"])
        s.add("dve", lambda e: e.tensor_tensor(out=lbsm[:], in0=lbe[:],
                                               in1=lbr[:].unsqueeze(2).broadcast_to([128, KC, DEPTH]), op=ALU.mult),
              r=["lbe", "lbr"], w=["lbsm"])
        s.add("pool", lambda e: e.memset(T["LB"][:, :, 0:1], 0.0), w=["LB"])
        for l in range(1, DEPTH):
            s.add("dve", (lambda l: lambda e: e.tensor_tensor(
                out=T["LB"][:, :, l:l + 1], in0=T["LB"][:, :, l - 1:l], in1=lbsm[:, :, l:l + 1], op=ALU.add))(l),
                r=["LB", "lbsm"], w=["LB"])
        s.add("dve", lambda e: e.tensor_scalar(out=T["OML"][:], in0=T["LB"][:], scalar1=-1.0, scalar2=1.0,
                                               op0=ALU.mult, op1=ALU.add), r=["LB"], w=["OML"])
        s.add("dve", lambda e: e.tensor_scalar(out=T["NOML"][:], in0=T["LB"][:], scalar1=1.0, scalar2=-1.0,
                                               op0=ALU.mult, op1=ALU.add), r=["LB"], w=["NOML"])

    def norm_mod(self, s, sb, ps, hT, Atab, Aname, Bcol0, l, h32=None):
        T, Sx = self.T, self.Sx
        xg = [sb("xg%d" % i, [128, KC, 512], F32) for i in range(2)]
        sq = [sb("sq%d" % i, [128, 512], F32) for i in range(2)]
        tmp = [sb("nt%d" % i, [128, 512], F32) for i in range(2)]
        rstd = [sb("rstd%d" % i, [128, 512], F32) for i in range(2)]
        pss = [ps("pssq%d" % i, [128, 512], F32) for i in range(2)]
        xt = Sx["XT"].rearrange("(kc p) t -> p kc t", p=128)
        def do(g):
            b = g % 2
            s.add("sp", (lambda b, g: lambda e: e.dma_start(out=xg[b][:], in_=xt[:, :, g * 512:(g + 1) * 512]))(b, g),
                  r=["XT"], w=["xg%d" % b], sem="dxg%d" % b)
            for kc in range(KC):
                q = kc % 2
                s.add("pool", (lambda b, kc, q: lambda e: e.tensor_tensor(
                    out=sq[q][:], in0=xg[b][:, kc, :], in1=xg[b][:, kc, :], op=ALU.mult))(b, kc, q),
                    r=["xg%d" % b], w=["sq%d" % q])
                s.add("pe", (lambda b, kc, q: lambda e: e.matmul(
                    pss[b][:], T["ones_f"][:, 0:128], sq[q][:], start=(kc == 0), stop=(kc == KC - 1)))(b, kc, q),
                    r=["sq%d" % q, "ones_f"], w=["pss%d" % b])
            s.add("act", (lambda b: lambda e: e.activation(
                out=rstd[b][:], in_=pss[b][:], func=AF.Ln, bias=self.cbias(float(D * EPS))))(b),
                r=["pss%d" % b], w=["rstd%d" % b])
            s.add("act", (lambda b: lambda e: e.activation(
                out=rstd[b][:], in_=rstd[b][:], func=AF.Exp, scale=-0.5))(b),
                r=["rstd%d" % b], w=["rstd%d" % b])
            for kc in range(KC):
                q = kc % 2
                s.add("dve", (lambda b, kc, q: lambda e: e.tensor_tensor(
                    out=tmp[q][:], in0=xg[b][:, kc, :], in1=rstd[b][:], op=ALU.mult))(b, kc, q),
                    r=["xg%d" % b, "rstd%d" % b], w=["nt%d" % q])
                if h32 is None:
                    s.add("act", (lambda g, kc, q: lambda e: e.activation(
                        out=hT[:, kc, g * 512:(g + 1) * 512], in_=tmp[q][:], func=AF.Identity,
                        bias=T["ADA"][:, l, Bcol0 + kc:Bcol0 + kc + 1], scale=Atab[:, l, kc:kc + 1]))(g, kc, q),
                        r=["nt%d" % q, "ADA", Aname], w=["hT%d" % g])
        return xg, do

    def phase_A(self, s, sb, ps):
        I, T, Sx = self.I, self.T, self.Sx
        l = self.l
        hT = sb("hT", [128, KC, S], BF16)
        xg, norm_group = self.norm_mod(s, sb, ps, hT, T["A1"], "A1", 0, l)
        norm_group(0)
        if self.stop_after == ("A1", l):
            for g in range(1, NG):
                norm_group(g)
            dbg = self.dscr("HT", [D, S], BF16)
            s.add("sp", lambda e: e.dma_start(out=dbg.rearrange("(kc p) t -> p kc t", p=128), in_=hT[:]),
                  r=["hT%d" % g for g in range(NG)], w=["HTd"], sem="dbg")
            return
        X = Ops(s)
        wb = Rot(sb, "wb", 2, [128, KC, 512], BF16)
        pacc = Rot(ps, "pacc", 4, [128, 512], F32)
        pn = Rot(ps, "pn", 2, [128, 512], F32)
        stg = Rot(sb, "stg", 8, [128, 512], BF16)
        f32t = Rot(sb, "ft", 12, [128, 512], F32)
        sqb = Rot(sb, "sqb", 2, [128, 512], BF16)
        vst = Rot(sb, "vst", 2, [128, 8, 65], BF16)
        for t_, n_ in vst.t:
            X.memset("pool", t_[:], 1.0, w=[n_])
        wff = sb("wff", [128, KC, 16], BF16)
        X.dma("pool", wff[:], I["w_ff"][l], r=[], w=["wff"])
        tsl = lambda g: slice(g * 512, (g + 1) * 512)

        def mm_fm(w_, wn, ci, g):
            p_, pn_ = pacc.get()
            for kc in range(KC):
                X.mm(p_[:], w_[:, kc, ci * 128:(ci + 1) * 128], hT[:, kc, tsl(g)], kc == 0, kc == KC - 1,
                     r=[wn, "hT%d" % g], w=[pn_])
            return p_, pn_

        def rsq(src, srcn, cb):
            r_, rn = f32t.get()
            X.act(r_[:], src, AF.Ln, r=[srcn, "cb"], w=[rn], bias=self.cbias(cb))
            X.act(r_[:], r_[:], AF.Exp, r=[rn], w=[rn], scale=-0.5)
            return r_, rn

        for gi in range(18):
            w_, wn = wb.get()
            X.dma("pool", w_[:], I["w_in"][l, gi], r=[], w=[wn])
            if gi < 4:
                isq = gi < 2
                dst = Sx["QT"] if isq else Sx["KT"]
                gcol = T["qgain"][:, l:l + 1] if isq else T["kgain8"][:, l:l + 1]
                for g in range(NG):
                    if gi == 0 and g + 1 < NG:
                        norm_group(g + 1)
                    for ci in range(4):
                        fc = (gi % 2) * 4 + ci
                        p_, pn_ = mm_fm(w_, wn, ci, g)
                        q_, qn_ = sqb.get()
                        X.act(q_[:], p_[:], AF.Square, r=[pn_], w=[qn_])
                        n_, nn_ = pn.get()
                        X.mm(n_[:], T["bd64"][:], q_[:], True, True, r=[qn_, "bd64"], w=[nn_])
                        r_, rn = rsq(n_[:], nn_, float(64 * EPS))
                        o_, on_ = stg.get()
                        X.stt("dve", o_[:], p_[:], gcol, r_[:], ALU.mult, ALU.mult, r=[pn_, rn, "gains"], w=[on_])
                        for i in range(2):
                            X.dma("sp", dst[2 * fc + i, 0:64, tsl(g)], o_[i * 64:(i + 1) * 64, :], r=[on_],
                                  w=["QT" if isq else "KT"], key="d_" + on_)
            elif gi < 8:
                for tb in range(NB):
                    p_, pn_ = pacc.get()
                    for kc in range(KC):
                        X.mm(p_[:], hT[:, kc, tb * 128:(tb + 1) * 128], w_[:, kc, :], kc == 0, kc == KC - 1,
                             r=[wn, "hT%d" % (tb // 4)], w=[pn_])
                    if gi < 6:
                        v_, vn_ = vst.get()
                        X.act(v_[:, :, 0:64], p_[:].rearrange("p (h d) -> p h d", d=64), AF.Copy, r=[pn_], w=[vn_])
                        h0 = (gi - 4) * 8
                        X.dma("sp", Sx["V"][tb * 128:(tb + 1) * 128, h0 * 65:(h0 + 8) * 65],
                              v_[:].rearrange("p h d -> p (h d)"), r=[vn_], w=["V"], key="d_" + vn_)
                    else:
                        o_, on_ = stg.get()
                        X.copy("dve", o_[:], p_[:], r=[pn_], w=[on_])
                        c0 = (gi - 6) * 512
                        X.dma("sp", Sx["VH"][tb * 128:(tb + 1) * 128, c0:c0 + 512], o_[:], r=[on_], w=["VH"],
                              key="d_" + on_)
                if gi == 5:
                    pf_, pfn_ = pacc.get()
                    for tb in range(NB):
                        for kc in range(KC):
                            X.mm(pf_[:, tb * 16:(tb + 1) * 16], hT[:, kc, tb * 128:(tb + 1) * 128], wff[:, kc, :],
                                 kc == 0, kc == KC - 1, r=["wff", "hT%d" % (tb // 4)], w=[pfn_])
                    t_, tn_ = f32t.get()
                    X.tt("dve", t_[:].rearrange("p (b h) -> p b h", h=16), pf_[:].rearrange("p (b h) -> p b h", h=16),
                         T["fbias"][:, l:l + 1, :].broadcast_to([128, NB, 16]), ALU.add, r=[pfn_, "fbias"], w=[tn_])
                    X.act(t_[:], t_[:], AF.Sigmoid, r=[tn_], w=[tn_])
                    X.act(T["LF"][:].rearrange("p b h -> p (b h)"), t_[:], AF.Ln, r=[tn_], w=["LF"])
            elif gi < 12:
                for g in range(NG):
                    for jj in range(2):
                        j = (gi - 8) * 2 + jj
                        pz, pzn = mm_fm(w_, wn, 2 * jj, g)
                        pq, pqn = mm_fm(w_, wn, 2 * jj + 1, g)
                        sig, sign = f32t.get()
                        X.act(sig[:], pz[:], AF.Sigmoid, r=[pzn], w=[sign])
                        sq_, sqn_ = f32t.get()
                        X.act(sq_[:], pq[:], AF.Sigmoid, r=[pqn], w=[sqn_])
                        X.tt("dve", sq_[:], pq[:], sq_[:], ALU.mult, r=[pqn, sqn_], w=[sqn_])
                        f_, fn_ = f32t.get()
                        X.ts("dve", f_[:], sig[:], T["OML"][:, j, l:l + 1], T["LB"][:, j, l:l + 1], ALU.mult, ALU.add,
                             r=[sign, "LBt"], w=[fn_])
                        X.s.add("dve", (lambda f_: lambda e: e.tensor_scalar_max(f_[:], f_[:], 1e-20))(f_), r=[fn_], w=[fn_])
                        X.act(f_[:], f_[:], AF.Ln, r=[fn_], w=[fn_])
                        kk, kkn = f32t.get()
                        X.ts("dve", kk[:], sig[:], T["NOML"][:, j, l:l + 1], T["OML"][:, j, l:l + 1], ALU.mult, ALU.add,
                             r=[sign, "LBt"], w=[kkn])
                        G_, Gn = f32t.get()
                        X.s.add("dve", (lambda G_, f_: lambda e: e.tensor_tensor_scan(
                            out=G_[:], data0=T["resetm"][:], data1=f_[:], initial=0.0, op0=ALU.mult, op1=ALU.add))(G_, f_),
                            r=[fn_, "resetm"], w=[Gn])
                        G3 = G_[:].rearrange("p (c i) -> p c i", i=CH)
                        d1, d1n = f32t.get()
                        X.tt("dve", d1[:].rearrange("p (c i) -> p c i", i=CH), G3, G3[:, :, CH // 2 - 1:CH // 2].broadcast_to([128, NCS, CH]),
                             ALU.subtract, r=[Gn], w=[d1n])
                        d3, d3n = f32t.get()
                        X.tt("dve", d3[:].rearrange("p (c i) -> p c i", i=CH), G3, G3[:, :, CH - 1:CH].broadcast_to([128, NCS, CH]),
                             ALU.subtract, r=[Gn], w=[d3n])
                        X.act(T["EGL"][:, j, g * NCS:(g + 1) * NCS], G3[:, :, CH - 1], AF.Exp, r=[Gn], w=["EGL"])
                        e1, e1n = f32t.get()
                        X.act(e1[:], d1[:], AF.Exp, r=[d1n], w=[e1n])
                        X.act(d1[:], d1[:], AF.Exp, r=[d1n], w=[d1n], scale=-1.0)
                        X.act(d3[:], d3[:], AF.Exp, r=[d3n], w=[d3n], scale=-1.0)
                        X.act(G_[:], G_[:], AF.Exp, r=[Gn], w=[Gn])
                        rows = slice(j * 128, (j + 1) * 128)
                        for (a_, an_, b_, bn_, dstn) in [(sq_, sqn_, e1, e1n, "QM"), (sq_, sqn_, G_, Gn, "QI"),
                                                         (kk, kkn, d1, d1n, "KM"), (kk, kkn, d3, d3n, "KL")]:
                            o_, on_ = stg.get()
                            X.tt("pool" if dstn in ("QM", "QI") else "dve", o_[:], a_[:], b_[:], ALU.mult, r=[an_, bn_], w=[on_])
                            X.dma("sp", Sx[dstn][rows, tsl(g)], o_[:], r=[on_], w=[dstn], key="d_" + on_)
            elif gi < 16:
                for g in range(NG):
                    for jj in range(2):
                        j = (gi - 12) * 2 + jj
                        pg_, pgn = mm_fm(w_, wn, 2 * jj, g)
                        pb_, pbn = mm_fm(w_, wn, 2 * jj + 1, g)
                        a_, an_ = f32t.get()
                        X.act(a_[:], pg_[:], AF.Sigmoid, r=[pgn], w=[an_])
                        b_, bn_ = f32t.get()
                        X.act(b_[:], pb_[:], AF.Sigmoid, r=[pbn], w=[bn_])
                        X.tt("dve", a_[:], pg_[:], a_[:], ALU.mult, r=[pgn, an_], w=[an_])
                        o_, on_ = stg.get()
                        X.tt("pool", o_[:], a_[:], b_[:], ALU.mult, r=[an_, bn_], w=[on_])
                        X.dma("sp", Sx["GG"][j * 128:(j + 1) * 128, tsl(g)], o_[:], r=[on_], w=["GG"], key="d_" + on_)
            else:
                for g in range(NG):
                    for ci in range(4):
                        fc = (gi - 16) * 4 + ci
                        p_, pn_ = mm_fm(w_, wn, ci, g)
                        o_, on_ = stg.get()
                        X.act(o_[:], p_[:], AF.Sigmoid, r=[pn_], w=[on_])
                        X.dma("sp", Sx["SGA"][fc * 128:(fc + 1) * 128, tsl(g)], o_[:], r=[on_], w=["SGA"], key="d_" + on_)

        lft = xg[0][0:16].rearrange("p k t -> p (k t)")
        ft = xg[1][0:16].rearrange("p k t -> p (k t)")
        rrow = sb("rrow", [16, S], BF16)
        for q4 in range(8):
            p_, pn_ = pacc.get()
            for i in range(4):
                tb = q4 * 4 + i
                X.s.add("pe", (lambda p_, i, tb: lambda e: e.transpose(
                    p_[0:16, i * 128:(i + 1) * 128], T["LF"][:, tb, :], T["ident_f"][:]))(p_, i, tb),
                    r=["LF", "ident_f"], w=[pn_])
            X.copy("dve", lft[:, q4 * 512:(q4 + 1) * 512], p_[0:16, :], r=[pn_], w=["xg0"])
        for q4 in range(8):
            ini = 0.0 if q4 == 0 else ft[:, q4 * 512 - 1:q4 * 512]
            X.s.add("dve", (lambda q4, ini: lambda e: e.tensor_tensor_scan(
                out=ft[:, q4 * 512:(q4 + 1) * 512], data0=T["ones_f"][0:16, :], data1=lft[:, q4 * 512:(q4 + 1) * 512],
                initial=ini, op0=ALU.mult, op1=ALU.add))(q4, ini), r=["xg0", "xg1"], w=["xg1"])
        X.memset("pool", rrow[:, 0:128], 0.0, w=["rrow"])
        X.copy("dve", rrow[:, 128:].rearrange("h (b i) -> h b i", i=128),
               ft[:, 0:31 * 128].rearrange("h (b i) -> h b i", i=128)[:, :, 127:128].broadcast_to([16, 31, 128]),
               r=["xg1", "rrow"], w=["rrow"])
        X.dma("sp", Sx["QT"][:, 64, :], rrow[:], r=["rrow"], w=["QT"], key="d_rrow")
        p_, pn_ = pacc.get()
        for tb in range(NB):
            X.s.add("pe", (lambda p_, tb: lambda e: e.transpose(
                p_[:, tb * 16:(tb + 1) * 16], ft[:, tb * 128:(tb + 1) * 128], T["ident_f"][0:16, 0:16]))(p_, tb),
                r=["xg1", "ident_f"], w=[pn_])
        X.s.add("dve", (lambda p_: lambda e: e.tensor_scalar(
            out=T["NF"][:].rearrange("p b h -> p (b h)"), in0=p_[:], scalar1=-1.0, scalar2=None, op0=ALU.mult))(p_),
            r=[pn_], w=["NF"])
        if "NFd" in self.dbg:
            nfd = self.dscr("NFd", [128, NB * 16], F32)
            X.dma("sp", nfd, T["NF"][:].rearrange("p b h -> p (b h)"), r=["NF"], w=["NFd"])
            egd = self.dscr("EGLd", [128, KC * (S // CH)], F32)
            X.dma("sp", egd, T["EGL"][:].rearrange("p j c -> p (j c)"), r=["EGL"], w=["EGLd"])


    def phase_H(self, s, sb, ps):
        I, T, Sx = self.I, self.T, self.Sx
        l = self.l
        X = Ops(s)
        ones_b = sb("ones_b", [128, 128], BF16)
        X.memset("pool", ones_b[:], 1.0, w=["ones_b"])
        segb = {nm: Rot(sb, "sg" + nm, 2, [128, KC, 512], BF16) for nm in ["QM", "KM", "QI", "KL", "GG"]}
        HV = NCS // 2
        vsg = Rot(sb, "vsg", 2, [CH, HV, D], BF16)
        St = sb("St", [128, KC, 128], F32)
        Sb = Rot(sb, "Sb", 2, [128, KC, 128], BF16)
        X.memset("pool", St[:], 0.0, w=["St%d" % j for j in range(KC)])
        sb0, sb0n = Sb.get()
        X.memset("pool", sb0[:], 0.0, w=[sb0n])
        pA = Rot(ps, "pA", 2, [CH, KC * CH], F32)
        pK = Rot(ps, "pK", 2, [CH, 1024], BF16)
        pU = Rot(ps, "pU", 2, [128, 512], F32)
        pO = Rot(ps, "pO", 1, [128, 512], F32)
        pN = Rot(ps, "pN", 1, [128, 512], F32)
        Am = Rot(sb, "Am", 2, [CH, KC, CH], BF16)
        kh = Rot(sb, "kh", 2, [CH, KC, 128], BF16)
        obuf = Rot(sb, "obuf", 2, [128, KC, 512], F32)
        osq = Rot(sb, "osq", 2, [128, 512], BF16)
        rt = Rot(sb, "hrt", 2, [128, 512], F32)
        t1 = Rot(sb, "ht1", 2, [128, 512], F32)
        ostg = Rot(sb, "ostg", 3, [128, 512], BF16)
        fmv = lambda nm: Sx[nm].rearrange("(j p) t -> p j t", p=128)
        cur = None

        def load_seg(sg):
            d = {}
            for nm in ["QM", "KM", "QI", "KL", "GG"]:
                t_, n_ = segb[nm].get()
                X.dma("sp", t_[:], fmv(nm)[:, :, sg * 512:(sg + 1) * 512], r=[nm], w=[n_])
                d[nm] = (t_, n_)
            return d

        def load_v(hv):
            v_, vn_ = vsg.get()
            X.dma("sp", v_[:], Sx["VH"][hv * 256:(hv + 1) * 256, :].rearrange("(c s) d -> s c d", s=CH), r=["VH"], w=[vn_])
            return (v_, vn_)

        def front(d, c):
            cs = slice(c * CH, (c + 1) * CH)
            pa, pan = pA.get()
            for j in range(KC):
                X.mm(pa[:, j * CH:(j + 1) * CH], d["KM"][0][:, j, cs], d["QM"][0][:, j, cs], True, True,
                     r=[d["KM"][1], d["QM"][1]], w=[pan])
            am, amn = Am.get()
            X.tt("dve", am[:], pa[:].rearrange("s (j t) -> s j t", t=CH),
                 T["tri64"][0:CH, 0:CH].unsqueeze(1).broadcast_to([CH, KC, CH]), ALU.mult, r=[pan], w=[amn])
            pk, pkn = pK.get()
            for j in range(KC):
                X.s.add("pe", (lambda pk, j, cs: lambda e: e.transpose(
                    pk[:, j * 128:(j + 1) * 128], d["KL"][0][:, j, cs], T["ident_b"][:]))(pk, j, cs),
                    r=[d["KL"][1]], w=[pkn])
            k_, kn_ = kh.get()
            X.act(k_[:].rearrange("s j k -> s (j k)"), pk[:], AF.Copy, r=[pkn], w=[kn_])
            return (am, amn, k_, kn_)

        sbc = (sb0, sb0n)
        nxt = load_seg(0)
        vnext = load_v(0)
        fr = front(nxt, 0)
        for sg in range(NG):
            d = nxt
            if sg + 1 < NG:
                nxt = load_seg(sg + 1)
            ob, obn = obuf.get()
            for c in range(NCS):
                cs = slice(c * CH, (c + 1) * CH)
                am, amn, k_, kn_ = fr
                if c % HV == 0:
                    v_, vn_ = vnext
                    hv = sg * 2 + c // HV
                    if hv + 1 < 2 * NG:
                        vnext = load_v(hv + 1)
                cv = c % HV
                pus = [pU.get(), pU.get()]
                for j in range(KC):
                    pu, pun = pus[j // 4]
                    X.mm(pu[:, (j % 4) * 128:(j % 4 + 1) * 128], k_[:, j, :], v_[:, cv, j * 128:(j + 1) * 128], True, True,
                         r=[kn_, vn_], w=[pun])
                if c + 1 < NCS:
                    fr = front(d, c + 1)
                elif sg + 1 < NG:
                    fr = front(nxt, 0)
                po, pon = pO.get()
                for j in range(KC):
                    X.mm(po[:, j * CH:(j + 1) * CH], v_[:, cv, j * 128:(j + 1) * 128], am[:, j, :], True, False,
                         r=[vn_, amn], w=[pon])
                    X.mm(po[:, j * CH:(j + 1) * CH], sbc[0][:, j, :], d["QI"][0][:, j, cs], False, True,
                         r=[sbc[1], d["QI"][1]], w=[pon])
                X.act(ob[:, :, cs], po[:, 0:KC * CH].rearrange("p (j t) -> p j t", t=CH), AF.Copy, r=[pon], w=[obn])
                cidx = sg * NCS + c
                for j in range(KC):
                    pu, pun = pus[j // 4]
                    X.stt("dve", St[:, j, :], St[:, j, :], T["EGL"][:, j, cidx:cidx + 1],
                          pu[:, (j % 4) * 128:(j % 4 + 1) * 128], ALU.mult, ALU.add, r=["St%d" % j, pun], w=["St%d" % j])
                sbc = Sb.get()
                X.act(sbc[0][:].rearrange("p j k -> p (j k)"), St[:].rearrange("p j k -> p (j k)"), AF.Copy,
                      r=["St%d" % j for j in range(KC)], w=[sbc[1]])
            for j in range(KC):
                q_, qn_ = osq.get()
                X.tt("pool", q_[:], ob[:, j, :], ob[:, j, :], ALU.mult, r=[obn], w=[qn_])
                n_, nn_ = pN.get()
                X.mm(n_[:], ones_b[:], q_[:], True, True, r=[qn_, "ones_b"], w=[nn_])
                r_, rn_ = rt.get()
                X.act(r_[:], n_[:], AF.Ln, r=[nn_], w=[rn_], bias=self.cbias(float(128 * EPS)))
                X.act(r_[:], r_[:], AF.Exp, r=[rn_], w=[rn_], scale=-0.5)
                a_, an_ = t1.get()
                X.stt("dve", a_[:], ob[:, j, :], T["ogainS"][:, l:l + 1], r_[:], ALU.mult, ALU.mult, r=[obn, rn_], w=[an_])
                o_, on_ = ostg.get()
                X.tt("dve", o_[:], a_[:], d["GG"][0][:, j, :], ALU.mult, r=[an_, d["GG"][1]], w=[on_])
                X.dma("sp", Sx["OBG"][j * 128:(j + 1) * 128, sg * 512:(sg + 1) * 512], o_[:], r=[on_], w=["OBG"],
                      key="d_" + on_)

    def phase_T(self, s, sb, ps):
        I, T, Sx = self.I, self.T, self.Sx
        l = self.l
        X = Ops(s)
        Vsb = sb("Vsb", [128, NB, 16 * 65], BF16)
        for q in range(4):
            X.dma("sp", Vsb[:, q * 8:(q + 1) * 8, :],
                  Sx["V"][q * 1024:(q + 1) * 1024, :].rearrange("(b p) c -> p b c", p=128), r=["V"], w=["Vsb%d" % q],
                  key="d_Vsb%d" % q)
        WO = sb("WO", [128, KC, D], BF16)
        X.dma("pool", WO[:], I["w_out"][l], r=[], w=["WO"])
        ktb = Rot(sb, "ktb", 2, [65, S], BF16)
        qtb = Rot(sb, "qtb", 2, [65, 512], BF16)
        ptb = Rot(sb, "ptb", 4, [128, 512], BF16)
        oab = Rot(sb, "oab", 2, [128, 4, D], BF16)
        rec = Rot(sb, "rec", 2, [128, 4], F32)
        pS = Rot(ps, "pS", 3, [128, 512], F32)
        pOo = Rot(ps, "pOo", 2, [128, 4, 65], F32)
        pTr = Rot(ps, "pTr", 1, [128, 512], BF16)
        pY = Rot(ps, "pY", 2, [128, 512], F32)
        sga = Rot(sb, "sga", 1, [128, KC, 512], BF16)
        obg = Rot(sb, "obg", 1, [128, KC, 512], BF16)
        mt = Rot(sb, "mt", 2, [128, 512], F32)
        yT = Rot(sb, "yT", 1, [128, KC, 512], BF16)
        xg = Rot(sb, "xgT", 1, [128, KC, 512], F32)
        fmv = lambda ap: ap.rearrange("(j p) t -> p j t", p=128)
        g1c = 16
        for G in range(NG):
            nk = 4 * G + 4
            oa, oan = oab.get()
            LA = 2
            tiles = [(h, j) for h in range(16) for j in range(nk)]
            hd = {}

            def load_head(h):
                kt, ktn = ktb.get()
                X.dma("sp", kt[:, 0:nk * 128], Sx["KT"][h, :, 0:nk * 128], r=["KT", "KT64"], w=[ktn])
                qt, qtn = qtb.get()
                X.dma("sp", qt[:], Sx["QT"][h, :, G * 512:(G + 1) * 512], r=["QT"], w=[qtn])
                hd[h] = [kt, ktn, qt, qtn, None]

            def qk(h, j):
                if j == 0:
                    if h + 1 < 16:
                        load_head(h + 1)
                kt, ktn, qt, qtn, _ = hd[h]
                nq0 = max(0, j - 4 * G)
                N = 512 - 128 * nq0
                p_, pn_ = pS.get()
                diag = j >= 4 * G
                if diag:
                    X.mm(p_[:, 0:N], T["ident_b"][:], T["maskneg"][:, 0:N], True, False, r=[], w=[pn_])
                X.mm(p_[:, 0:N], kt[:, j * 128:(j + 1) * 128], qt[:, nq0 * 128:512], not diag, True,
                     r=[ktn, qtn], w=[pn_])
                return (p_, pn_, nq0, N)

            load_head(0)
            pend = [qk(*tiles[i]) for i in range(min(LA, len(tiles)))]
            for i, (h, j) in enumerate(tiles):
                if i + LA < len(tiles):
                    pend.append(qk(*tiles[i + LA]))
                p_, pn_, nq0, N = pend.pop(0)
                if j == 0:
                    hd[h][4] = pOo.get()
                po, pon = hd[h][4]
                pt, ptn = ptb.get()
                X.act(pt[:, 0:N], p_[:, 0:N], AF.Exp, r=[pn_, "NF"], w=[ptn], bias=T["NF"][:, j, h:h + 1])
                for qb in range(nq0, 4):
                    X.mm(po[:, qb, :], pt[:, (qb - nq0) * 128:(qb - nq0 + 1) * 128], Vsb[:, j, h * 65:(h + 1) * 65],
                         j == 0 and qb == 0, j == 4 * G + qb, r=[ptn, "Vsb%d" % (j // 8)], w=[pon], skip=True)
                if j == nk - 1:
                    rc, rcn = rec.get()
                    X.s.add("dve", (lambda rc, po: lambda e: e.reciprocal(rc[:], po[:, :, 64]))(rc, po), r=[pon], w=[rcn])
                    X.tt("dve", oa[:, :, h * 64:(h + 1) * 64], po[:, :, 0:64], rc[:].unsqueeze(2).broadcast_to([128, 4, 64]),
                         ALU.mult, r=[pon, rcn], w=[oan])
                    del hd[h]
            if "OAd" in self.dbg:
                if G == 0:
                    self.oad = self.dscr("OAd", [S, D], BF16)
                X.dma("sp", self.oad[G * 512:(G + 1) * 512, :].rearrange("(q p) d -> p q d", p=128), oa[:], r=[oan],
                      w=["OAd"], key="d_oad")
            tsl = slice(G * 512, (G + 1) * 512)
            sg_, sgn = sga.get()
            X.dma("sp", sg_[:], fmv(Sx["SGA"])[:, :, tsl], r=["SGA"], w=[sgn])
            ob_, obn = obg.get()
            X.dma("sp", ob_[:], fmv(Sx["OBG"])[:, :, tsl], r=["OBG"], w=[obn])
            x_, xn_ = xg.get()
            X.dma("sp", x_[:], fmv(Sx["XT"])[:, :, tsl], r=["XT"], w=[xn_])
            y_, yn_ = yT.get()
            for fc in range(KC):
                tr, trn = pTr.get()
                for qb in range(4):
                    X.s.add("pe", (lambda tr, qb, fc, oa: lambda e: e.transpose(
                        tr[:, qb * 128:(qb + 1) * 128], oa[:, qb, fc * 128:(fc + 1) * 128], T["ident_b"][:]))(tr, qb, fc, oa),
                        r=[oan], w=[trn])
                m_, mn_ = mt.get()
                X.tt("dve", m_[:], tr[:], sg_[:, fc, :], ALU.mult, r=[trn, sgn], w=[mn_])
                X.tt("pool", y_[:, fc, :], m_[:], ob_[:, fc, :], ALU.add, r=[mn_, obn], w=[yn_])
            for dc in range(KC):
                py, pyn = pY.get()
                for fc in range(KC):
                    X.mm(py[:], WO[:, fc, dc * 128:(dc + 1) * 128], y_[:, fc, :], fc == 0, fc == KC - 1,
                         r=["WO", yn_], w=[pyn])
                X.stt("dve", x_[:, dc, :], py[:], T["ADA"][:, l, g1c + dc:g1c + dc + 1], x_[:, dc, :], ALU.mult, ALU.add,
                      r=[pyn, xn_], w=[xn_])
            X.dma("sp", fmv(Sx["XT"])[:, :, tsl], x_[:], r=[xn_], w=["XT"], key="d_st" + xn_)

    def phase_M(self, s, sb, ps):
        I, T, Sx = self.I, self.T, self.Sx
        l = self.l
        X = Ops(s)
        moe = (l % 2 == 1)
        li = l // 2
        NFC = 28 if moe else 22
        ne = NE if moe else 1
        last = (l == self.n_layers - 1)
        fmv = lambda ap: ap.rearrange("(j p) t -> p j t", p=128)
        hT = sb("hT2", [128, KC, 1024], BF16)
        xa = sb("xa", [128, KC, 1024], F32)
        hid = sb("hid", [128, NFC, 1024], BF16)
        sqr = Rot(sb, "msq", 2, [128, 512], F32)
        nt = Rot(sb, "mnt", 2, [128, 512], F32)
        rstd = Rot(sb, "mrs", 1, [128, 512], F32)
        pss = Rot(ps, "mpss", 1, [128, 512], F32)
        pG = Rot(ps, "pG", 2, [128, 512], F32)
        pUu = Rot(ps, "pUu", 2, [128, 512], F32)
        pD = Rot(ps, "pD", 2, [128, 512], F32)
        wg = Rot(sb, "wg", 2, [128, KC, 128], BF16)
        wu = Rot(sb, "wu", 2, [128, KC, 128], BF16)
        wd = Rot(sb, "wd", 2, [128, NFC, 128], BF16)
        wgs = Rot(sb, "wgs", 2, [128, KC, 128], F32)
        wus = Rot(sb, "wus", 2, [128, KC, 128], F32)
        NH = NFC // 2
        wds = Rot(sb, "wds", 2, [128, NH, 128], F32)
        sgt = Rot(sb, "sgt", 2, [128, 512], F32)
        tmpd = Rot(sb, "tmpd", 2, [128, 512], F32)
        if moe:
            wr = sb("wr", [128, KC, NE], F32)
            X.dma("sp", wr[:], I["w_router"][li], r=[], w=["wr"])
            h32 = Rot(sb, "h32", 1, [128, 512], F32)
            pL = Rot(ps, "pL", 1, [128, 512], F32)
            CT = sb("CT", [8, 1024], F32)
            CBr = Rot(sb, "CBe", 2, [128, 1024], BF16)
            lg = sb("lg", [128, 8, NE], F32)
            small = {nm: sb("sm_" + nm, [128, 8, NE], F32) for nm in ["eq", "l2", "sel", "ex", "w"]}
            m1 = sb("m1", [128, 8], F32)
            m2 = sb("m2", [128, 8], F32)
            ssum = sb("ssum", [128, 8], F32)
        A2, Bc, g2c = T["A2"], 24, 40
        for SG in range(4):
            tsl = slice(SG * 1024, (SG + 1) * 1024)
            X.dma("sp", xa[:], fmv(Sx["XT"])[:, :, tsl], r=["XT"], w=["xa"])
            if moe:
                pl, pln = pL.get()
            for hf in range(2):
                hs = slice(hf * 512, (hf + 1) * 512)
                p_, pn_ = pss.get()
                for kc in range(KC):
                    q_, qn_ = sqr.get()
                    X.tt("pool", q_[:], xa[:, kc, hs], xa[:, kc, hs], ALU.mult, r=["xa"], w=[qn_])
                    X.mm(p_[:], T["ones_f"][:, 0:128], q_[:], kc == 0, kc == KC - 1, r=[qn_], w=[pn_])
                r_, rn_ = rstd.get()
                X.act(r_[:], p_[:], AF.Ln, r=[pn_], w=[rn_], bias=self.cbias(float(D * EPS)))
                X.act(r_[:], r_[:], AF.Exp, r=[rn_], w=[rn_], scale=-0.5)
                for kc in range(KC):
                    t_, tn_ = nt.get()
                    X.tt("dve", t_[:], xa[:, kc, hs], r_[:], ALU.mult, r=["xa", rn_], w=[tn_])
                    X.act(hT[:, kc, hs], t_[:], AF.Identity, r=[tn_], w=["hT2_%d" % hf],
                          bias=T["ADA"][:, l, Bc + kc:Bc + kc + 1], scale=A2[:, l, kc:kc + 1])
                    if moe:
                        h_, hn_ = h32.get()
                        X.ts("dve", h_[:], t_[:], A2[:, l, kc:kc + 1], T["ADA"][:, l, Bc + kc:Bc + kc + 1], ALU.mult, ALU.add,
                             r=[tn_], w=[hn_])
                        for tb in range(4):
                            col = (hf * 4 + tb) * NE
                            X.mm(pl[:, col:col + NE], h_[:, tb * 128:(tb + 1) * 128], wr[:, kc, :],
                                 hf == 0 and kc == 0 and tb == 0, kc == KC - 1, r=[hn_, "wr"], w=[pln], skip=True)
            if moe:
                v3 = lambda t: t[:]
                bc = lambda t: t[:].unsqueeze(2).broadcast_to([128, 8, NE])
                X.tt("dve", lg[:], pl[:, 0:64].rearrange("p (b e) -> p b e", e=NE),
                     T["brout"][:, li:li + 1, :].broadcast_to([128, 8, NE]), ALU.add, r=[pln], w=["lg"])
                X.s.add("dve", lambda e: e.tensor_reduce(out=m1[:], in_=lg[:], axis=AX.X, op=ALU.max), r=["lg"], w=["m1"])
                X.tt("dve", small["eq"][:], lg[:], bc(m1), ALU.is_equal, r=["lg", "m1"], w=["eq"])
                X.stt("dve", small["l2"][:], small["eq"][:], -1e30, lg[:], ALU.mult, ALU.add, r=["eq", "lg"], w=["l2"])
                X.s.add("dve", lambda e: e.tensor_reduce(out=m2[:], in_=small["l2"][:], axis=AX.X, op=ALU.max), r=["l2"], w=["m2"])
                X.tt("dve", small["sel"][:], lg[:], bc(m2), ALU.is_ge, r=["lg", "m2"], w=["sel"])
                X.tt("dve", small["ex"][:], lg[:], bc(m1), ALU.subtract, r=["lg", "m1"], w=["ex"])
                X.act(small["ex"][:], small["ex"][:], AF.Exp, r=["ex"], w=["ex"])
                X.tt("dve", small["w"][:], small["ex"][:], small["sel"][:], ALU.mult, r=["ex", "sel"], w=["w"])
                X.s.add("dve", lambda e: e.tensor_reduce(out=ssum[:], in_=small["w"][:], axis=AX.X, op=ALU.add), r=["w"], w=["ssum"])
                X.s.add("dve", lambda e: e.reciprocal(ssum[:], ssum[:]), r=["ssum"], w=["ssum"])
                X.tt("dve", small["w"][:], small["w"][:], bc(ssum), ALU.mult, r=["w", "ssum"], w=["w"])
                for hf in range(2):
                    p_, pn_ = pD.get()
                    for tb in range(4):
                        X.s.add("pe", (lambda p_, tb, hf: lambda e: e.transpose(
                            p_[0:8, tb * 128:(tb + 1) * 128], small["w"][:, hf * 4 + tb, :], T["ident_f"][:]))(p_, tb, hf),
                            r=["w"], w=[pn_])
                    X.copy("dve", CT[:, hf * 512:(hf + 1) * 512], p_[0:8, :], r=[pn_], w=["CT"])
            for e_ in range(ne):
                if moe:
                    CB, cbn = CBr.get()
                    for hf in range(2):
                        p_, pn_ = pD.get()
                        X.mm(p_[:], T["sel8"][:, e_, :], CT[:, hf * 512:(hf + 1) * 512], True, True, r=["CT"], w=[pn_])
                        X.copy("dve", CB[:, hf * 512:(hf + 1) * 512], p_[:], r=[pn_], w=[cbn])
                for fcn in range(NFC):
                    g32, g32n = wgs.get()
                    u32, u32n = wus.get()
                    X.dma("sp", g32[:], (I["mwg"][li, e_, fcn] if moe else I["dwg"][li, fcn]), r=[], w=[g32n])
                    X.dma("sp", u32[:], (I["mwu"][li, e_, fcn] if moe else I["dwu"][li, fcn]), r=[], w=[u32n])
                    g_, gn_ = wg.get()
                    u_, un_ = wu.get()
                    X.act(g_[:], g32[:], AF.Copy, r=[g32n], w=[gn_])
                    X.copy("dve", u_[:], u32[:], r=[u32n], w=[un_])
                    for hf in range(2):
                        hs = slice(hf * 512, (hf + 1) * 512)
                        pg, pgn = pG.get()
                        for kc in range(KC):
                            X.mm(pg[:], g_[:, kc, :], hT[:, kc, hs], kc == 0, kc == KC - 1, r=[gn_, "hT2_%d" % hf], w=[pgn])
                        pu, pun = pUu.get()
                        for kc in range(KC):
                            X.mm(pu[:], u_[:, kc, :], hT[:, kc, hs], kc == 0, kc == KC - 1, r=[un_, "hT2_%d" % hf], w=[pun])
                        sg_, sgn = sgt.get()
                        X.act(sg_[:], pg[:], AF.Silu, r=[pgn], w=[sgn])
                        X.tt("dve", hid[:, fcn, hs], pu[:], sg_[:], ALU.mult, r=[pun, sgn], w=["hid"])
                for dc in range(KC):
                    d_, dn_ = wd.get()
                    src = (I["mwd"][li, e_][:, :, dc * 128:(dc + 1) * 128] if moe else I["dwd"][li][:, :, dc * 128:(dc + 1) * 128])
                    for hh in range(2):
                        d32, d32n = wds.get()
                        X.dma("sp", d32[:], src[:, hh * NH:(hh + 1) * NH, :], r=[], w=[d32n])
                        if hh == 0:
                            X.copy("dve", d_[:, hh * NH:(hh + 1) * NH, :], d32[:], r=[d32n], w=[dn_])
                        else:
                            X.act(d_[:, hh * NH:(hh + 1) * NH, :], d32[:], AF.Copy, r=[d32n], w=[dn_])
                    for hf in range(2):
                        hs = slice(hf * 512, (hf + 1) * 512)
                        pd, pdn = pD.get()
                        for fcn in range(NFC):
                            X.mm(pd[:], d_[:, fcn, :], hid[:, fcn, hs], fcn == 0, fcn == NFC - 1, r=[dn_, "hid"], w=[pdn])
                        gcol = T["ADA"][:, l, g2c + dc:g2c + dc + 1]
                        if moe:
                            t_, tn_ = tmpd.get()
                            X.stt("dve", t_[:], pd[:], gcol, CB[:, hs], ALU.mult, ALU.mult, r=[pdn, cbn], w=[tn_])
                            X.tt("pool", xa[:, dc, hs], xa[:, dc, hs], t_[:], ALU.add, r=[tn_, "xa"], w=["xa"])
                        else:
                            X.stt("dve", xa[:, dc, hs], pd[:], gcol, xa[:, dc, hs], ALU.mult, ALU.add, r=[pdn, "xa"], w=["xa"])
            dst = self.out if last else Sx["XT"]
            X.dma("sp", fmv(dst)[:, :, tsl], xa[:], r=["xa"], w=["XT"], key="d_stxa")


class Rot:
    def __init__(self, alloc, name, n, shape, dt):
        self.t = [(alloc("%s%d" % (name, i), shape, dt), "%s%d" % (name, i)) for i in range(n)]
        self.i = 0

    def get(self):
        t = self.t[self.i % len(self.t)]
        self.i += 1
        return t


class Ops:
    def __init__(self, s):
        self.s = s

    def mm(self, out, lhsT, rhs, start, stop, r, w, skip=False):
        if skip:
            self.s.add("pe", lambda e: e.matmul(out, lhsT, rhs, start=start, stop=stop, skip_group_check=True), r=r, w=w)
        else:
            self.s.add("pe", lambda e: e.matmul(out, lhsT, rhs, start=start, stop=stop), r=r, w=w)

    def act(self, out, in_, func, r, w, bias=None, scale=None):
        kw = {}
        if bias is not None:
            kw["bias"] = bias
        if scale is not None:
            kw["scale"] = scale
        self.s.add("act", lambda e: e.activation(out=out, in_=in_, func=func, **kw), r=r, w=w)

    def tt(self, eng, out, in0, in1, op, r, w):
        self.s.add(eng, lambda e: e.tensor_tensor(out=out, in0=in0, in1=in1, op=op), r=r, w=w)

    def ts(self, eng, out, in0, s1, s2, op0, op1, r, w):
        self.s.add(eng, lambda e: e.tensor_scalar(out=out, in0=in0, scalar1=s1, scalar2=s2, op0=op0, op1=op1), r=r, w=w)

    def stt(self, eng, out, in0, scalar, in1, op0, op1, r, w):
        self.s.add(eng, lambda e: e.scalar_tensor_tensor(out=out, in0=in0, scalar=scalar, in1=in1, op0=op0, op1=op1),
                   r=r, w=w)

    def copy(self, eng, out, in_, r, w):
        self.s.add(eng, lambda e: e.tensor_copy(out, in_), r=r, w=w)

    def memset(self, eng, ap, v, w):
        self.s.add(eng, lambda e: e.memset(ap, v), w=w)

    def dma(self, eng, out, in_, r, w, key=None):
        if key is None:
            key = "d_" + w[0]
        self.s.add(eng, lambda e: e.dma_start(out=out, in_=in_), r=r, w=w, sem=key)


def _consts():
    ident = np.eye(128, dtype=np.float32)
    bd64 = np.zeros((128, 128), np.float32)
    bd64[:64, :64] = 1.0
    bd64[64:, 64:] = 1.0
    ss, tt = np.meshgrid(np.arange(64), np.arange(64), indexing="ij")
    tri64 = (ss <= tt).astype(np.float32)
    maskneg = np.zeros((128, 512), np.float32)
    ss, tt = np.meshgrid(np.arange(128), np.arange(128), indexing="ij")
    maskneg[:, :128] = np.where(ss > tt, -30000.0, 0.0)
    resetm = np.ones((128, 512), np.float32)
    resetm[:, ::CH] = 0.0
    sel8 = np.zeros((8, NE, 128), np.float32)
    for e in range(NE):
        sel8[e, e, :] = 1.0
    return dict(ident=ident, bd64=bd64, tri64=tri64, maskneg=maskneg, resetm=resetm, sel8=sel8)


def _pf(a):
    a = np.asarray(a, np.float32)
    lead = a.shape[:-1]
    n = a.shape[-1] // 128
    a = a.reshape(lead + (n, 128))
    return np.ascontiguousarray(np.moveaxis(a, -1, 0))


def _wtiles(w, ncol):
    K, N = w.shape
    kc = K // 128
    return np.ascontiguousarray(w.reshape(kc, 128, N // ncol, ncol).transpose(2, 1, 0, 3))


def _layout_fns(inp):
    f = lambda k: np.asarray(inp[k], np.float32)
    o = {}
    o["w_ada"] = lambda: np.stack([_wtiles(f("w_ada")[l], 768) for l in range(DEPTH)])
    o["b_ada"] = lambda: np.ascontiguousarray(_pf(f("b_ada")))
    o["norm_mix"] = lambda: _pf(f("norm_mix"))
    o["norm_ffn"] = lambda: _pf(f("norm_ffn"))

    def w_in_groups():
        w_in = f("w_in")
        offs = np.cumsum([0, 1024, 1024, 1024, 16, 1024, 1024, 1024, 1024, 1024, 1024])
        fq, fk, fv, ff, hq, hf, hi, hg, ga, gb = [w_in[:, :, offs[i]:offs[i + 1]] for i in range(10)]
        groups = [fq[:, :, :512], fq[:, :, 512:], fk[:, :, :512], fk[:, :, 512:], fv[:, :, :512], fv[:, :, 512:],
                  hi[:, :, :512], hi[:, :, 512:]]
        sl = lambda a, j: a[:, :, j * 128:(j + 1) * 128]
        for j in range(0, 8, 2):
            groups.append(np.concatenate([sl(hf, j), sl(hq, j), sl(hf, j + 1), sl(hq, j + 1)], axis=2))
        for j in range(0, 8, 2):
            groups.append(np.concatenate([sl(hg, j), sl(gb, j), sl(hg, j + 1), sl(gb, j + 1)], axis=2))
        groups += [ga[:, :, :512], ga[:, :, 512:]]
        wr = np.concatenate(groups, axis=2)
        return np.stack([_wtiles(wr[l], 512) for l in range(DEPTH)])

    o["w_in"] = w_in_groups
    o["w_ff"] = lambda: np.ascontiguousarray(f("w_in")[:, :, 3072:3088].reshape(DEPTH, KC, 128, 16).transpose(0, 2, 1, 3))
    o["qgain"] = lambda: np.ascontiguousarray(np.tile(f("fox_q_gain"), (1, 2)).T)
    o["kgain"] = lambda: np.ascontiguousarray(np.tile(f("fox_k_gain"), (1, 2)).T)
    o["fbias"] = lambda: np.ascontiguousarray(np.broadcast_to(f("fox_f_bias")[None], (128, DEPTH, 16)))
    o["hg_lb"] = lambda: np.ascontiguousarray(_pf(f("hg_lb")).transpose(0, 2, 1))
    o["ogain"] = lambda: np.ascontiguousarray(f("hg_o_gain").T)
    o["w_out"] = lambda: np.ascontiguousarray(f("w_out").reshape(DEPTH, KC, 128, D).transpose(0, 2, 1, 3))
    o["w_router"] = lambda: np.ascontiguousarray(f("w_router").reshape(2, KC, 128, NE).transpose(0, 2, 1, 3))
    o["b_router"] = lambda: np.ascontiguousarray(np.broadcast_to(f("b_router")[None], (128, 2, NE)))
    o["dwg"] = lambda: np.stack([_wtiles(f("dense_w_gate")[i], 128) for i in range(2)])
    o["dwu"] = lambda: np.stack([_wtiles(f("dense_w_up")[i], 128) for i in range(2)])
    o["dwd"] = lambda: np.ascontiguousarray(f("dense_w_down").reshape(2, 22, 128, D).transpose(0, 2, 1, 3))
    o["mwg"] = lambda: np.stack([np.stack([_wtiles(f("moe_w_gate")[i, e], 128) for e in range(NE)]) for i in range(2)])
    o["mwu"] = lambda: np.stack([np.stack([_wtiles(f("moe_w_up")[i, e], 128) for e in range(NE)]) for i in range(2)])
    o["mwd"] = lambda: np.ascontiguousarray(f("moe_w_down").reshape(2, NE, 28, 128, D).transpose(0, 1, 3, 2, 4))
    for k, v in _consts().items():
        o[k] = (lambda v: lambda: v)(v)
    return o


def _layout_shared_subset(inp, keys):
    fns = _layout_fns(inp)
    return {k: fns[k]() for k in keys if k in fns}


def _layout_shared(inp):
    fns = _layout_fns(inp)
    return {k: fn() for k, fn in fns.items()}


def make_in_maps(inp, n_cores=8):
    shared = _layout_shared(inp)
    x = np.asarray(inp["x"], np.float32)
    c = np.asarray(inp["c"], np.float32)
    maps = []
    for b in range(n_cores):
        m = dict(shared)
        m["xT"] = np.ascontiguousarray(x[b].T)
        m["c"] = np.ascontiguousarray(c[b].reshape(KC, 128).T)
        maps.append(m)
    return maps


def kernel(**inp):
    mk = MK()
    nc = mk.build()
    maps = make_in_maps(inp)
    res = run_bass_kernel_spmd(nc, maps, core_ids=list(range(8)))
    out = np.stack([np.ascontiguousarray(r["yT"].T) for r in res.results], axis=0)
    return out.astype(np.float32)
```
